# Optimizing a Trainium2 kernel written in Bass

```python
import math
import jax, jax.numpy as jnp
from jax import lax
import numpy as np

D_MODEL = 1024
BATCH = 8
SEQ = 2048
DEPTH = 1

PLE_DIM = 256
HEAD_DIM = 64
D_MIX = D_MODEL
SB_HEADS = D_MIX // 2 // HEAD_DIM
RET_HEADS = D_MIX // 2 // HEAD_DIM
SB_WIDTH = SB_HEADS * HEAD_DIM
RET_WIDTH = RET_HEADS * HEAD_DIM
D_IN = 3 * SB_WIDTH + 4 * RET_WIDTH
Q_BLOCK = 128
RET_CHUNK = 128
ROPE_BASE = 10000.0
N_EXPERTS = 32
TOP_K = 4
D_EXPERT = D_MODEL
SWIGLU_LIMIT = 7.0
SWIGLU_ALPHA = 1.702
MOE_BLOCK = 128
EPS = 1e-6

kernel_name = "hybrid_sbattn_retention_moe_ple"


def _rmsnorm(x, g):
    xf = x.astype(jnp.float32)
    y = xf * lax.rsqrt(jnp.mean(xf * xf, axis=-1, keepdims=True) + EPS)
    return (y * g.astype(jnp.float32)).astype(x.dtype)


def _heads(t, n_heads):
    b, s, _ = t.shape
    return t.reshape(b, s, n_heads, HEAD_DIM).transpose(0, 2, 1, 3)


def _rope(t, positions):
    half = HEAD_DIM // 2
    inv_freq = ROPE_BASE ** (-jnp.arange(half, dtype=jnp.float32) / half)
    ang = positions.astype(jnp.float32)[:, None, :, None] * inv_freq
    cos, sin = jnp.cos(ang), jnp.sin(ang)
    tf = t.astype(jnp.float32)
    t1, t2 = tf[..., :half], tf[..., half:]
    return jnp.concatenate([t1 * cos - t2 * sin, t2 * cos + t1 * sin], axis=-1).astype(t.dtype)


def _stick_breaking(q, k, v):
    b, h, s, d = q.shape
    nqb = s // Q_BLOCK
    scale = 1.0 / math.sqrt(d)
    qs = q.reshape(b, h, nqb, Q_BLOCK, d).transpose(2, 0, 1, 3, 4)
    t0s = jnp.arange(nqb, dtype=jnp.int32) * Q_BLOCK
    kpos = jnp.arange(s, dtype=jnp.int32)

    def block(args):
        qb, t0 = args
        z = jnp.einsum('bhqd,bhkd->bhqk', qb, k).astype(jnp.float32) * scale
        qpos = t0 + jnp.arange(Q_BLOCK, dtype=jnp.int32)
        mask = kpos[None, :] < qpos[:, None]
        lneg = jnp.where(mask, jax.nn.log_sigmoid(-z), 0.0)
        tail = lax.cumsum(lneg, axis=3, reverse=True) - lneg
        w = jnp.where(mask, jnp.exp(jax.nn.log_sigmoid(z) + tail), 0.0)
        return jnp.einsum('bhqk,bhkd->bhqd', w.astype(v.dtype), v)

    out = lax.map(block, (qs, t0s))
    return out.transpose(1, 2, 0, 3, 4).reshape(b, h, s, d)


def _retention(q, k, v):
    b, h, s, d = q.shape
    n = s // RET_CHUNK
    c = RET_CHUNK
    log_g = jnp.log(1.0 - 2.0 ** (-5.0 - jnp.arange(h, dtype=jnp.float32)))
    idx = jnp.arange(c, dtype=jnp.float32)
    diff = idx[:, None] - idx[None, :]
    dmat = jnp.where(diff >= 0, jnp.exp(log_g[:, None, None] * jnp.maximum(diff, 0.0)), 0.0)
    k_dec = jnp.exp(log_g[:, None] * (c - 1 - idx)[None, :])
    q_dec = jnp.exp(log_g[:, None] * (idx + 1.0)[None, :])
    g_chunk = jnp.exp(log_g * c)
    qc = q.reshape(b, h, n, c, d)
    kc = k.reshape(b, h, n, c, d)
    vc = v.reshape(b, h, n, c, d)
    dt = q.dtype
    scores = jnp.einsum('bhnid,bhnjd->bhnij', qc, kc) * dmat[None, :, None].astype(dt)
    inner = jnp.einsum('bhnij,bhnjd->bhnid', scores, vc)
    kv = jnp.einsum('bhnjd,bhnje->bhnde', kc * k_dec[None, :, None, :, None].astype(dt), vc)

    def step(state, kv_n):
        return g_chunk[:, None, None].astype(state.dtype) * state + kv_n, state

    init = jnp.zeros((b, h, d, d), dtype=kv.dtype)
    _, prev = lax.scan(step, init, jnp.moveaxis(kv, 2, 0))
    prev = jnp.moveaxis(prev, 0, 2)
    cross = jnp.einsum('bhnid,bhnde->bhnie', qc, prev) * q_dec[None, :, None, :, None].astype(dt)
    return (inner + cross).reshape(b, h, s, d)


def _group_norm(y, gain, bias):
    yf = y.astype(jnp.float32)
    mu = jnp.mean(yf, axis=-1, keepdims=True)
    var = jnp.mean((yf - mu) ** 2, axis=-1, keepdims=True)
    yn = (yf - mu) * lax.rsqrt(var + EPS)
    b, h, s, d = y.shape
    yn = yn.transpose(0, 2, 1, 3).reshape(b, s, h * d)
    return (yn * gain.astype(jnp.float32) + bias.astype(jnp.float32)).astype(y.dtype)


def _moe(xt, w_router, b_router, w_up_gate, b_up_gate, w_down, b_down):
    t, d = xt.shape
    logits = (xt @ w_router + b_router).astype(jnp.float32)
    top_vals, top_idx = lax.top_k(logits, TOP_K)
    gates = jax.nn.softmax(top_vals, axis=-1)
    a = t * TOP_K
    flat_e = top_idx.reshape(a).astype(jnp.int32)
    flat_tok = jnp.arange(a, dtype=jnp.int32) // TOP_K
    flat_gate = gates.reshape(a)
    order = jnp.argsort(flat_e)
    sorted_e = flat_e[order]
    counts = jnp.zeros((N_EXPERTS,), jnp.int32).at[flat_e].add(1)
    padded = ((counts + MOE_BLOCK - 1) // MOE_BLOCK) * MOE_BLOCK
    pad_end = jnp.cumsum(padded)
    pad_start = pad_end - padded
    start = jnp.cumsum(counts) - counts
    rank = jnp.arange(a, dtype=jnp.int32) - start[sorted_e]
    dest = pad_start[sorted_e] + rank
    n_slots = a + N_EXPERTS * MOE_BLOCK
    n_blocks = n_slots // MOE_BLOCK
    slot_tok = jnp.zeros((n_slots,), jnp.int32).at[dest].set(flat_tok[order])
    slot_gate = jnp.zeros((n_slots,), jnp.float32).at[dest].set(flat_gate[order])
    block_e = jnp.clip(jnp.searchsorted(pad_end, jnp.arange(n_blocks, dtype=jnp.int32) * MOE_BLOCK, side='right'), 0, N_EXPERTS - 1)
    xs = xt[slot_tok].reshape(n_blocks, MOE_BLOCK, d)

    def expert_block(args):
        xb, e = args
        hgu = xb @ w_up_gate[e] + b_up_gate[e]
        x_glu = jnp.minimum(hgu[:, :D_EXPERT], SWIGLU_LIMIT)
        x_lin = jnp.clip(hgu[:, D_EXPERT:], -SWIGLU_LIMIT, SWIGLU_LIMIT)
        hact = x_glu * jax.nn.sigmoid(SWIGLU_ALPHA * x_glu) * (x_lin + 1.0)
        return hact @ w_down[e] + b_down[e]

    ys = lax.map(expert_block, (xs, block_e)).reshape(n_slots, d)
    return jnp.zeros((t, d), xt.dtype).at[slot_tok].add(ys * slot_gate[:, None].astype(xt.dtype))


def setup_inputs(seed: int = 0) -> dict:
    key = jax.random.key(seed)
    ks = jax.random.split(key, 24)
    nrm = lambda k, shape, s: jax.random.normal(k, shape, jnp.float32) * s
    L = DEPTH
    return {
        "x": nrm(ks[0], (BATCH, SEQ, D_MODEL), 1.0),
        "p": nrm(ks[1], (DEPTH, BATCH, SEQ, PLE_DIM), 1.0),
        "positions": jnp.broadcast_to(jnp.arange(SEQ, dtype=jnp.int32), (BATCH, SEQ)),
        "g_mix_norm": 1.0 + nrm(ks[2], (L, D_MODEL), 0.02),
        "w_in": nrm(ks[3], (L, D_MODEL, D_IN), D_MODEL ** -0.5),
        "sb_q_gain": 1.0 + nrm(ks[4], (L, HEAD_DIM), 0.02),
        "sb_k_gain": 1.0 + nrm(ks[5], (L, HEAD_DIM), 0.02),
        "ret_q_gain": 1.0 + nrm(ks[6], (L, HEAD_DIM), 0.02),
        "ret_k_gain": 1.0 + nrm(ks[7], (L, HEAD_DIM), 0.02),
        "ret_gn_gain": 1.0 + nrm(ks[8], (L, RET_WIDTH), 0.02),
        "ret_gn_bias": nrm(ks[9], (L, RET_WIDTH), 0.02),
        "w_out": nrm(ks[10], (L, D_MIX, D_MODEL), D_MIX ** -0.5),
        "g_ffn_norm": 1.0 + nrm(ks[11], (L, D_MODEL), 0.02),
        "w_router": nrm(ks[12], (L, D_MODEL, N_EXPERTS), D_MODEL ** -0.5),
        "b_router": nrm(ks[13], (L, N_EXPERTS), 0.01),
        "w_up_gate": nrm(ks[14], (L, N_EXPERTS, D_MODEL, 2 * D_EXPERT), D_MODEL ** -0.5),
        "b_up_gate": nrm(ks[15], (L, N_EXPERTS, 2 * D_EXPERT), 0.02),
        "w_down": nrm(ks[16], (L, N_EXPERTS, D_EXPERT, D_MODEL), D_EXPERT ** -0.5),
        "b_down": nrm(ks[17], (L, N_EXPERTS, D_MODEL), 0.02),
        "g_ple_norm": 1.0 + nrm(ks[18], (L, D_MODEL), 0.02),
        "w_ple_gate": nrm(ks[19], (L, D_MODEL, D_MODEL), D_MODEL ** -0.5),
        "w_ple_proj": nrm(ks[20], (L, PLE_DIM, D_MODEL), PLE_DIM ** -0.5),
    }


def reference(x, p, positions, g_mix_norm, w_in, sb_q_gain, sb_k_gain, ret_q_gain, ret_k_gain,
              ret_gn_gain, ret_gn_bias, w_out, g_ffn_norm, w_router, b_router, w_up_gate, b_up_gate,
              w_down, b_down, g_ple_norm, w_ple_gate, w_ple_proj):
    b, s, d = x.shape
    for i in range(DEPTH):
        xn = _rmsnorm(x, g_mix_norm[i])
        proj = xn @ w_in[i]
        o = 0
        sb_q = proj[..., o:o + SB_WIDTH]; o += SB_WIDTH
        sb_k = proj[..., o:o + SB_WIDTH]; o += SB_WIDTH
        sb_v = proj[..., o:o + SB_WIDTH]; o += SB_WIDTH
        r_q = proj[..., o:o + RET_WIDTH]; o += RET_WIDTH
        r_k = proj[..., o:o + RET_WIDTH]; o += RET_WIDTH
        r_v = proj[..., o:o + RET_WIDTH]; o += RET_WIDTH
        r_g = proj[..., o:o + RET_WIDTH]
        q_a = _rmsnorm(_heads(sb_q, SB_HEADS), sb_q_gain[i])
        k_a = _rmsnorm(_heads(sb_k, SB_HEADS), sb_k_gain[i])
        y_a = _stick_breaking(q_a, k_a, _heads(sb_v, SB_HEADS))
        y_a = y_a.transpose(0, 2, 1, 3).reshape(b, s, SB_WIDTH)
        q_b = _rope(_rmsnorm(_heads(r_q, RET_HEADS), ret_q_gain[i]), positions)
        k_b = _rope(_rmsnorm(_heads(r_k, RET_HEADS), ret_k_gain[i]), positions) * (HEAD_DIM ** -0.5)
        y_b = _retention(q_b, k_b, _heads(r_v, RET_HEADS))
        y_b = _group_norm(y_b, ret_gn_gain[i], ret_gn_bias[i]) * jax.nn.silu(r_g)
        x = x + jnp.concatenate([y_a, y_b], axis=-1) @ w_out[i]
        hn = _rmsnorm(x, g_ffn_norm[i]).reshape(b * s, d)
        x = x + _moe(hn, w_router[i], b_router[i], w_up_gate[i], b_up_gate[i], w_down[i], b_down[i]).reshape(b, s, d)
        gate = jax.nn.sigmoid(_rmsnorm(x, g_ple_norm[i]) @ w_ple_gate[i])
        x = x + (p[i] @ w_ple_proj[i]) * gate
    return x
```

```python
import math
from contextlib import ExitStack

import numpy as np
import concourse.bass as bass
import concourse.mybir as mybir
from concourse.bass_utils import run_bass_kernel_spmd

F32 = mybir.dt.float32
BF16 = mybir.dt.bfloat16
I32 = mybir.dt.int32
AF = mybir.ActivationFunctionType
ALU = mybir.AluOpType
AX = mybir.AxisListType

D = 1024
S = 2048
NT = 16
KC = 8
DIN = 3584
HD = 64
NH = 8
NE = 32
TOPK = 4
PLE = 256
EPS = 1e-6
LIMIT = 7.0
ALPHA = 1.702

ENGS = ["pe", "act", "dve", "pool", "sp"]


class Op:
    __slots__ = ("eng", "fn", "deps", "is_dma", "signal", "count", "sem", "semval", "prewait", "phase")

    def __init__(self, eng, fn, deps, is_dma, phase):
        self.eng = eng
        self.fn = fn
        self.deps = deps
        self.is_dma = is_dma
        self.signal = False
        self.count = None
        self.sem = None
        self.semval = None
        self.prewait = None
        self.phase = phase


class Slot:
    __slots__ = ("w", "r", "w2")

    def __init__(self):
        self.w = None
        self.r = []
        self.w2 = None


class Ctx:
    def __init__(self, nc, es):
        self.nc = nc
        self.esem = {}
        self.ecount = {}
        for e in ["pe", "act", "dve", "pool"]:
            self.esem[e] = es.enter_context(nc.semaphore("es_" + e))
            self.ecount[e] = 0
        self.NDS = 6
        self.dsem = {}
        self.dval = {}
        self.didx = {}
        for e in ["sp", "act", "pool"]:
            self.dsem[e] = [es.enter_context(nc.semaphore("ds_%s%d" % (e, i))) for i in range(self.NDS)]
            self.dval[e] = [0] * self.NDS
            self.didx[e] = 0
        self.phase_id = 0


class Phase:
    def __init__(self, K, name):
        self.K = K
        self.name = name
        self.ops = {e: [] for e in ENGS}
        K.phase_id += 1
        self.id = K.phase_id

    def _add(self, eng, fn, deps, is_dma):
        dl = []
        for d in deps:
            if d is None or d.phase != self.id:
                continue
            if d.eng == "pe" and eng == "pe" and not d.is_dma and not is_dma:
                continue
            if not d.is_dma:
                d.signal = True
            dl.append(d)
        op = Op(eng, fn, dl, is_dma, self.id)
        self.ops[eng].append(op)
        return op

    def op(self, eng, fn, reads=(), writes=(), extra=(), r=None, w=None, x=None):
        reads = r if r is not None else reads
        writes = w if w is not None else writes
        extra = x if x is not None else extra
        deps = list(extra)
        for s in reads:
            deps.append(s.w)
        for s in writes:
            deps.append(s.w)
            deps.extend(s.r)
        o = self._add(eng, fn, deps, False)
        for s in reads:
            s.r.append(o)
        for s in writes:
            s.w = o
            s.r = []
        return o

    def dma(self, eng, fn, reads=(), writes=(), extra=()):
        deps = list(extra)
        for s in reads:
            deps.append(s.w)
        for s in writes:
            deps.append(s.w)
            deps.extend(s.r)
        o = self._add(eng, fn, deps, True)
        for s in reads:
            s.r.append(o)
        for s in writes:
            s.w = o
            s.r = []
        return o

    def emit(self):
        K = self.K
        nc = K.nc
        for e in ENGS:
            for o in self.ops[e]:
                if o.is_dma:
                    j = K.didx[e] % K.NDS
                    K.didx[e] += 1
                    o.sem = K.dsem[e][j]
                    o.prewait = K.dval[e][j]
                    K.dval[e][j] += 16
                    o.semval = K.dval[e][j]
                elif o.signal:
                    K.ecount[e] += 1
                    o.count = K.ecount[e]
        with nc.Block() as block:
            def run(e, engobj):
                waited = {}

                def wait(sem, val):
                    key = id(sem)
                    if waited.get(key, 0) >= val:
                        return
                    engobj.wait_ge(sem, val)
                    waited[key] = val

                for o in self.ops[e]:
                    for d in o.deps:
                        if d.is_dma:
                            wait(d.sem, d.semval)
                        else:
                            wait(K.esem[d.eng], d.count)
                    if o.is_dma:
                        if o.prewait > 0:
                            wait(o.sem, o.prewait)
                        o.fn(engobj).then_inc(o.sem, 16)
                    else:
                        inst = o.fn(engobj)
                        if o.signal:
                            inst.then_inc(K.esem[e], 1)

            if self.ops["pe"]:
                block.tensor(lambda eng: run("pe", eng))
            if self.ops["act"]:
                block.scalar(lambda eng: run("act", eng))
            if self.ops["dve"]:
                block.vector(lambda eng: run("dve", eng))
            if self.ops["pool"]:
                block.gpsimd(lambda eng: run("pool", eng))
            if self.ops["sp"]:
                block.sync(lambda eng: run("sp", eng))


def slots(n):
    return [Slot() for _ in range(n)]


def build_nc(debug=None, POOL="pool", BSTOP=99):
    nc = bass.Bass("TRN2", target_bir_lowering=False)

    SHAPES = {
        "x": ([S, D], F32), "p": ([S, PLE], F32), "pos_col": ([128, NT], I32), "gmix_col": ([128, KC], F32),
        "w_in": ([D, DIN], F32), "gains_bc": ([128, 4, 512], F32), "gn_bc": ([128, 2, 512], F32),
        "w_out": ([D, D], F32), "gffn_col": ([128, KC], F32), "w_router": ([D, NE], F32),
        "b_router_bc": ([128, NE], F32), "w_up_gate": ([NE, D, 2 * D], F32), "b_ug_col": ([128, NE, 16], F32),
        "w_down": ([NE, D, D], F32), "b_down": ([NE, D], F32), "gple_col": ([128, KC], F32),
        "w_ple_gate": ([D, D], F32), "w_ple_proj": ([PLE, D], F32),
        "c_ident": ([128, 128], F32), "c_invf": ([128, 32], F32), "c_qdec": ([128, NH], F32),
        "c_kinv": ([128, NH], F32), "c_gcb": ([128, 4, 64], F32), "c_retmask": ([128, 128], F32),
        "c_negtri": ([128, 128], F32), "c_negones": ([128, 128], F32), "c_sbmask": ([128, 4, 512], F32),
    }
    declared = {}

    def din(name):
        if name not in declared:
            shape, dt = SHAPES[name]
            declared[name] = nc.dram_tensor(name, list(shape), dt, kind="ExternalInput").ap()
        return declared[name]

    out_d = nc.dram_tensor("out", [S, D], F32, kind="ExternalOutput").ap()
    dbg = {"_declared": declared}

    def dout(name, shape, dt=F32):
        t = nc.dram_tensor(name, list(shape), dt, kind="ExternalOutput").ap()
        dbg[name] = t
        return t

    with ExitStack() as es:
        K = Ctx(nc, es)
        ARENA_K = 190
        arena = es.enter_context(nc.sbuf_tensor("arena", [128, ARENA_K * 256], F32))

        def V(off_k, shape, dt):
            n = 1
            for d_ in shape[1:]:
                n *= d_
            off = int(round(off_k * 256))
            if dt == BF16:
                assert n % 2 == 0
                a = arena[:, off:off + n // 2].bitcast(BF16)
                assert off + n // 2 <= ARENA_K * 256
            else:
                a = arena[:, off:off + n]
                if dt != F32:
                    a = a.bitcast(dt)
                assert off + n <= ARENA_K * 256
            if len(shape) == 3:
                a = a.rearrange("p (a b) -> p a b", a=shape[1])
            elif len(shape) == 4:
                a = a.rearrange("p (a b c) -> p a b c", a=shape[1], b=shape[2])
            return a

        def sb(name, shape, dt, stack=es):
            return stack.enter_context(nc.sbuf_tensor(name, list(shape), dt))[:]

        def ps(name, shape, dt, stack=es):
            n = 1
            for d_ in shape[1:]:
                n *= d_
            nbytes = n * (2 if dt == BF16 else 4)
            nbanks = (nbytes + 2047) // 2048
            t = stack.enter_context(nc.psum_tensor(name, [128, 512 * nbanks], F32))[:]
            a = t[:, 0:nbytes // 4]
            if dt == BF16:
                a = a.bitcast(BF16)
            if len(shape) == 3:
                a = a.rearrange("p (a b) -> p a b", a=shape[1])
            if shape[0] != 128:
                a = a[0:shape[0]]
            return a

        def ACT(P, out, in_, func, r=(), w=(), x=(), **kw):
            return P.op("act", lambda e: e.activation(out=out, in_=in_, func=func, **kw), reads=r, writes=w, extra=x)

        def TT(P, eng, out, in0, in1, op, r=(), w=(), x=()):
            return P.op(eng, lambda e: e.tensor_tensor(out=out, in0=in0, in1=in1, op=op), reads=r, writes=w, extra=x)

        def TS(P, eng, out, in0, s1, s2, op0, op1=None, r=(), w=(), x=()):
            if op1 is None:
                return P.op(eng, lambda e: e.tensor_scalar(out=out, in0=in0, scalar1=s1, scalar2=None, op0=op0),
                            reads=r, writes=w, extra=x)
            return P.op(eng, lambda e: e.tensor_scalar(out=out, in0=in0, scalar1=s1, scalar2=s2, op0=op0, op1=op1),
                        reads=r, writes=w, extra=x)

        def STT(P, out, in0, sc, in1, op0, op1, r=(), w=(), x=()):
            return P.op("dve", lambda e: e.scalar_tensor_tensor(out=out, in0=in0, scalar=sc, in1=in1, op0=op0, op1=op1),
                        reads=r, writes=w, extra=x)

        def CP(P, eng, out, in_, r=(), w=(), x=()):
            return P.op(eng, lambda e: e.tensor_copy(out=out, in_=in_), reads=r, writes=w, extra=x)

        def RED(P, out, in_, r=(), w=(), x=()):
            return P.op("dve", lambda e: e.tensor_reduce(out=out, in_=in_, axis=AX.X, op=ALU.add), reads=r, writes=w,
                        extra=x)

        def RCP(P, out, in_, r=(), w=(), x=()):
            return P.op("dve", lambda e: e.reciprocal(out=out, in_=in_), reads=r, writes=w, extra=x)

        def MM(P, out, lhsT, rhs, start, stop, r=(), w=(), x=(), sgc=False):
            return P.op("pe", lambda e: e.matmul(out, lhsT, rhs, start=start, stop=stop, skip_group_check=sgc),
                        reads=r, writes=w, extra=x)

        def TR(P, out, in_, idn, r=(), w=(), x=()):
            return P.op("pe", lambda e: e.transpose(out=out, in_=in_, identity=idn), reads=r, writes=w, extra=x)

        def DMA(P, out, in_, r=(), w=(), x=(), eng="sp"):
            return P.dma(eng, lambda e: e.dma_start(out=out, in_=in_), reads=r, writes=w, extra=x)

        def finish(P, fin):
            P.op("sp", lambda e: e.nop(), extra=fin)

        def load_w_bf16(P, dst, src, ncols, nkc, wst, s_wst, s_dst, cnt, scale_col=None, eng="dve", kstep=None):
            kstep = kstep or wst[0].shape[1]
            for k0 in range(0, nkc, kstep):
                b = cnt[0] % len(wst)
                cnt[0] += 1
                DMA(P, wst[b][:, 0:kstep, 0:ncols], src[:, k0:k0 + kstep, :], w=[s_wst[b]])
                for j in range(kstep):
                    kc = k0 + j
                    if scale_col is not None:
                        TS(P, eng, dst[:, kc, :], wst[b][:, j, 0:ncols], scale_col[:, kc:kc + 1], None, ALU.mult,
                           r=[s_wst[b]], w=[s_dst[kc]])
                    else:
                        CP(P, eng, dst[:, kc, :], wst[b][:, j, 0:ncols], r=[s_wst[b]], w=[s_dst[kc]])

        ident = sb("ident", [128, 128], F32)
        identb = sb("identb", [128, 128], BF16)
        rstd_x = sb("rstd_x", [128, NT], F32)
        qdec = sb("qdec", [128, NH], F32)
        kinv = sb("kinv", [128, NH], F32)
        gmix = sb("gmix", [128, KC], F32)
        G = sb("G", [128, NT, NE], F32)

        yT = V(0, [128, KC, S], BF16)
        xT = V(32, [128, KC, S], BF16)
        gains = V(64, [128, 4, 512], F32)
        cosT = V(72, [128, NT, 32], F32)
        sinT = V(74, [128, NT, 32], F32)

        P = Phase(K, "p1")
        with ExitStack() as e1:
            pos_i = sb("pos_i", [128, NT], I32, e1)
            posf = sb("posf", [128, NT], F32, e1)
            invf = sb("invf", [128, 32], F32, e1)
            ang = V(76, [128, NT, 32], F32)
            ufr = V(78, [128, NT, 32], F32)
            uint_ = V(80, [128, NT, 32], I32)
            uflo = V(82, [128, NT, 32], F32)
            umask = V(84, [128, NT, 32], F32)
            xt = [V(86 + 4 * i, [128, D], F32) for i in range(2)]
            junk = V(94, [128, D], F32)
            ssx = sb("ssx", [128, NT], F32, e1)
            sqx = sb("sqx", [128, NT], F32, e1)
            tp = [ps("tp%d" % i, [128, 4, 128], F32, e1) for i in range(4)]

            cl = []
            for (dst, src) in [(ident, din("c_ident")), (pos_i, din("pos_col")), (invf, din("c_invf")),
                               (gains, din("gains_bc")), (qdec, din("c_qdec")), (kinv, din("c_kinv")),
                               (gmix, din("gmix_col"))]:
                cl.append(DMA(P, dst, src))
            o = CP(P, "dve", identb, ident, x=cl)
            s_ident = Slot(); s_ident.w = cl[0]
            o1 = TS(P, "dve", gains[:, 0, :], gains[:, 0, :], 0.125, None, ALU.mult, x=cl)
            o2 = TS(P, "dve", gains[:, 3, :], gains[:, 3, :], 0.125, None, ALU.mult, x=cl + [o1])
            o = CP(P, "dve", posf, pos_i, x=cl + [o2])
            o = TT(P, "dve", ang, posf[:, :, None].broadcast_to([128, NT, 32]),
                   invf[:, None, :].broadcast_to([128, NT, 32]), ALU.mult, x=[o])
            last = o
            for (dst, off) in [(sinT, 0.5), (cosT, 0.75)]:
                o = TS(P, "dve", ufr, ang, 1.0 / (2 * math.pi), off, ALU.mult, ALU.add, x=[last])
                o = CP(P, "dve", uint_, ufr, x=[o])
                o = CP(P, "dve", uflo, uint_, x=[o])
                o = TT(P, "dve", ufr, ufr, uflo, ALU.subtract, x=[o])
                o = TS(P, "dve", umask, ufr, 0.0, None, ALU.is_lt, x=[o])
                o = TT(P, "dve", ufr, ufr, umask, ALU.add, x=[o])
                o = TS(P, "dve", ufr, ufr, 2 * math.pi, -math.pi, ALU.mult, ALU.add, x=[o])
                o = TS(P, "dve", ufr, ufr, -3.14159, 3.14159, ALU.max, ALU.min, x=[o])
                o = ACT(P, dst, ufr, AF.Sin, x=[o])
                last = o

            s_xt = slots(2)
            s_tp = slots(4)
            s_junk = Slot()
            x_d = din("x")
            fin1 = [last]
            for t in range(NT):
                b = t % 2
                DMA(P, xt[b], x_d[t * 128:(t + 1) * 128, :], w=[s_xt[b]])
                ACT(P, junk, xt[b], AF.Square, r=[s_xt[b]], w=[s_junk], accum_out=ssx[:, t:t + 1])
                for half in range(2):
                    pi = (t * 2 + half) % 4
                    for j in range(4):
                        kc = half * 4 + j
                        o = TR(P, tp[pi][:, j, :], xt[b][:, kc * 128:(kc + 1) * 128], ident,
                               r=[s_xt[b], s_ident], w=([s_tp[pi]] if j == 0 else []))
                    s_tp[pi].w = o
                    fin1.append(CP(P, "dve", xT[:, half * 4:half * 4 + 4, t * 128:(t + 1) * 128], tp[pi],
                                   r=[s_tp[pi]]))
            o = ACT(P, sqx, ssx, AF.Sqrt, x=[s_junk.w], scale=1.0 / D, bias=EPS)
            o = RCP(P, rstd_x, sqx, x=[o])
            P.emit()

        sbqT = V(76, [128, 4, S], BF16)
        sbkT = V(92, [128, 4, S], BF16)
        sbv = V(108, [128, NT, 512], BF16)
        w_in_v = din("w_in").rearrange("(kc p) n -> p kc n", p=128)

        class QKTmp:
            def __init__(self, base_k, stack, tag, n=2):
                self.n = n
                self.qs = [V(base_k + 2 * i, [128, 512], F32) for i in range(n)]
                self.sq = [V(base_k + 2 * n + 2 * i, [128, 512], F32) for i in range(n)]
                self.qn = [V(base_k + 4 * n + 2 * i, [128, 512], F32) for i in range(n)]
                self.ra = [V(base_k + 6 * n + i, [128, 8, 32], F32) for i in range(n)]
                self.rb = [V(base_k + 7 * n + i, [128, 8, 32], F32) for i in range(n)]
                self.ssh = [sb("ssh%s%d" % (tag, i), [128, 8], F32, stack) for i in range(n)]
                self.srt = [sb("srt%s%d" % (tag, i), [128, 8], F32, stack) for i in range(n)]
                self.rsh = [sb("rsh%s%d" % (tag, i), [128, 8], F32, stack) for i in range(n)]
                self.s = {k: slots(n) for k in ["qs", "sq", "qn", "ra", "rb", "ssh", "srt", "rsh"]}
                self.size_k = 8 * n

        def post_qk(P, T_, i, pj_ap, s_pj, t, gi, rope, dec, out_bf, s_out):
            rc = rstd_x[:, t:t + 1]
            s = T_.s
            ACT(P, T_.qs[i], pj_ap, AF.Copy, r=[s_pj], w=[s["qs"][i]], scale=rc)
            ACT(P, T_.sq[i], pj_ap, AF.Square, r=[s_pj], w=[s["sq"][i]], scale=rc)
            RED(P, T_.ssh[i], T_.sq[i].rearrange("p (h d) -> p h d", h=8), r=[s["sq"][i]], w=[s["ssh"][i]])
            ACT(P, T_.srt[i], T_.ssh[i], AF.Sqrt, r=[s["ssh"][i]], w=[s["srt"][i]], scale=1.0 / HD, bias=EPS)
            RCP(P, T_.rsh[i], T_.srt[i], r=[s["srt"][i]], w=[s["rsh"][i]])
            if dec is not None:
                TT(P, "dve", T_.rsh[i], T_.rsh[i], dec, ALU.mult, w=[s["rsh"][i]])
            q3 = T_.qn[i].rearrange("p (h d) -> p h d", h=8)
            TT(P, "dve", q3, T_.qs[i].rearrange("p (h d) -> p h d", h=8),
               T_.rsh[i][:, :, None].broadcast_to([128, 8, 64]), ALU.mult,
               r=[s["qs"][i], s["rsh"][i]], w=[s["qn"][i]])
            if not rope:
                TT(P, "dve", out_bf, T_.qn[i], gains[:, gi, :], ALU.mult, r=[s["qn"][i]], w=[s_out])
                return
            TT(P, "dve", T_.qn[i], T_.qn[i], gains[:, gi, :], ALU.mult, w=[s["qn"][i]])
            prev = [s_out.w, s_out.w2] + list(s_out.r)
            o3 = out_bf.rearrange("p (h d) -> p h d", h=8)
            cb = cosT[:, t:t + 1, :].broadcast_to([128, 8, 32])
            sbb = sinT[:, t:t + 1, :].broadcast_to([128, 8, 32])
            t1 = q3[:, :, 0:32]
            t2 = q3[:, :, 32:64]
            TT(P, POOL, T_.ra[i], t1, cb, ALU.mult, r=[s["qn"][i]], w=[s["ra"][i]])
            TT(P, POOL, T_.rb[i], t2, sbb, ALU.mult, r=[s["qn"][i]], w=[s["rb"][i]])
            TT(P, POOL, o3[:, :, 0:32], T_.ra[i], T_.rb[i], ALU.subtract, r=[s["ra"][i], s["rb"][i]], w=[s_out])
            TT(P, "dve", T_.ra[i], t2, cb, ALU.mult, r=[s["qn"][i]], w=[s["ra"][i]])
            TT(P, "dve", T_.rb[i], t1, sbb, ALU.mult, r=[s["qn"][i]], w=[s["rb"][i]])
            o = TT(P, "dve", o3[:, :, 32:64], T_.ra[i], T_.rb[i], ALU.add, r=[s["ra"][i], s["rb"][i]], x=prev)
            s_out.r.append(o)
            s_out.w2 = o

        P = Phase(K, "A1")
        with ExitStack() as e2:
          if debug != "Bonly":
                wst = [V(124 + 8 * i, [128, 4, 512], F32) for i in range(2)]
                s_wst = slots(2)
                Wg = [V(140 + 8 * i, [128, 8, 512], BF16) for i in range(2)]
                s_Wg = [slots(8) for _ in range(2)]
                T_ = QKTmp(156, e2, "a")
                qbf = [V(172 + i, [128, 512], BF16) for i in range(2)]
                s_qbf = slots(2)
                pj = [ps("pjA%d" % i, [128, 512], F32, e2) for i in range(2)]
                s_pj = slots(2)
                tpb = [ps("tpbA%d" % i, [128, 4, 128], BF16, e2) for i in range(2)]
                s_tpb = slots(2)
                cnt = [0]
                n_i = 0
                for cg in range(3):
                    wb = cg % 2
                    load_w_bf16(P, Wg[wb], w_in_v[:, :, cg * 512:(cg + 1) * 512], 512, 8, wst, s_wst, s_Wg[wb], cnt,
                                scale_col=gmix)
                    def projA(t, wb=wb):
                        pb_ = t % 2
                        for kc in range(KC):
                            o_ = MM(P, pj[pb_], xT[:, kc, t * 128:(t + 1) * 128], Wg[wb][:, kc, :], kc == 0, kc == KC - 1,
                                    r=[s_Wg[wb][kc]], w=([s_pj[pb_]] if kc == 0 else []))
                        s_pj[pb_].w = o_

                    projA(0)
                    for t in range(NT):
                        pb = t % 2
                        if t + 1 < NT:
                            projA(t + 1)
                        if cg < 2:
                            i = n_i % 2
                            n_i += 1
                            post_qk(P, T_, i, pj[pb], s_pj[pb], t, cg, False, None, qbf[i], s_qbf[i])
                            for j in range(4):
                                o = TR(P, tpb[i][:, j, :], qbf[i][:, j * 128:(j + 1) * 128], identb,
                                       r=[s_qbf[i]], w=([s_tpb[i]] if j == 0 else []))
                            s_tpb[i].w = o
                            dst = (sbqT if cg == 0 else sbkT)[:, :, t * 128:(t + 1) * 128]
                            CP(P, "dve", dst, tpb[i], r=[s_tpb[i]])
                        else:
                            ACT(P, sbv[:, t, :], pj[pb], AF.Copy, r=[s_pj[pb]], scale=rstd_x[:, t:t + 1])
                P.emit()

        if debug == "A1":
            P = Phase(K, "dbgA1")
            fin = [DMA(P, dout("d_sbqT", [128, 4, S], BF16), sbqT), DMA(P, dout("d_sbkT", [128, 4, S], BF16), sbkT),
                   DMA(P, dout("d_sbv", [128, NT, 512], BF16), sbv)]
            finish(P, fin)
            P.emit()
            return nc, dbg

        P = Phase(K, "A2")
        with ExitStack() as e3:
          if debug != "Bonly":
                NSP = 4
                sp_bf = [V(124 + i, [128, 512], BF16) for i in range(NSP)]
                S_bf = [V(128 + i, [128, 512], BF16) for i in range(NSP)]
                acc = V(132, [128, 512], F32)
                wb_ = [V(134 + i, [128, 512], BF16) for i in range(3)]
                etmp = [V(137 + 2 * i, [128, 512], F32) for i in range(2)]
                sbmask = V(141, [128, 4, 512], BF16)
                negtri = V(145, [128, 128], BF16)
                negones = V(145.25, [128, 128], BF16)
                cst = V(146, [128, 4, 512], F32)
                cst2 = V(154, [128, 2, 128], F32)
                NZ = 5
                z_ps = [ps("z_ps%d" % i, [128, 512], F32, e3) for i in range(NZ)]
                y_ps = [ps("y_ps%d" % i, [128, 512], F32, e3) for i in range(2)]
                junk_ps = ps("junk_ps", [128, 512], F32, e3)
                s_z = slots(NZ); s_y = slots(2); s_w = slots(3); s_et = slots(2)
                s_sp = slots(NSP); s_S = slots(NSP); s_acc = Slot()
                d1 = DMA(P, cst, din("c_sbmask"))
                d2 = DMA(P, cst2[:, 0, :], din("c_negtri"))
                d3 = DMA(P, cst2[:, 1, :], din("c_negones"))
                s_cm = Slot(); s_cm.w = CP(P, "dve", sbmask, cst, x=[d1])
                s_nt = Slot(); s_nt.w = CP(P, "dve", negtri, cst2[:, 0, :], x=[d2])
                s_no = Slot(); s_no.w = CP(P, "dve", negones, cst2[:, 1, :], x=[d3])
                tiles = []
                u = 0
                for hp in range(4):
                    for hh in range(2):
                        for b in range(4):
                            Tn = 4 * b + 4
                            for idx in range(Tn):
                                kt = Tn - 1 - idx
                                tiles.append(dict(hp=hp, hh=hh, h=2 * hp + hh, pb=hh * 64, b=b, Tn=Tn, idx=idx, kt=kt,
                                                  band=(kt >= 4 * b), mi=kt - 4 * b, ub=u % 2, n=len(tiles),
                                                  c0=(128 * (kt - 4 * b) if kt >= 4 * b else 0)))
                            u += 1

                def filler(k=1):
                    for _ in range(k):
                        MM(P, junk_ps, negones, sbmask[:, 0, :], True, True, r=[s_no, s_cm])

                def qk(tl, dst, s_dst, last):
                    pb, hp, b, kt, c0 = tl["pb"], tl["hp"], tl["b"], tl["kt"], tl["c0"]
                    o_ = MM(P, dst[:, c0:], sbkT[pb:pb + 64, hp, kt * 128:(kt + 1) * 128],
                            sbqT[pb:pb + 64, hp, b * 512 + c0:(b + 1) * 512], True, last and not tl["band"], w=[s_dst])
                    if tl["band"]:
                        o_ = MM(P, dst[:, c0:], identb, sbmask[:, tl["mi"], c0:], False, last, r=[s_cm])
                    return o_

                def stage1(tl):
                    n, c0 = tl["n"], tl["c0"]
                    zi, ei, si = n % NZ, n % 2, n % NSP
                    o_ = qk(tl, z_ps[zi], s_z[zi], True)
                    s_z[zi].w = o_
                    filler(1)
                    ACT(P, etmp[ei][:, c0:], z_ps[zi][:, c0:], AF.Exp, r=[s_z[zi]], w=[s_et[ei]])
                    ACT(P, sp_bf[si][:, c0:], etmp[ei][:, c0:], AF.Ln, r=[s_et[ei]], w=[s_sp[si]], bias=1.0)
                    if tl["idx"] < tl["Tn"] - 1:
                        if tl["idx"] == 0:
                            P.op("dve", lambda e, c0=c0: e.memset(acc[:, 0:c0], 0.0), w=[s_acc])
                            CP(P, "dve", acc[:, c0:], sp_bf[si][:, c0:], r=[s_sp[si]], w=[s_acc])
                        else:
                            TT(P, "dve", acc[:, c0:], acc[:, c0:], sp_bf[si][:, c0:], ALU.add, r=[s_sp[si]], w=[s_acc])
                        CP(P, "dve", S_bf[si], acc, r=[s_acc], w=[s_S[si]])

                def stage2(tl):
                    n, c0 = tl["n"], tl["c0"]
                    zi, wi, si = n % NZ, n % 3, n % NSP
                    first = tl["idx"] == 0
                    o_ = MM(P, z_ps[zi][:, c0:], negtri, sp_bf[si][:, c0:], False, first, r=[s_nt, s_sp[si]],
                            w=[s_z[zi]], sgc=True)
                    if not first:
                        sp_prev = (n - 1) % NSP
                        o_ = MM(P, z_ps[zi][:, c0:], negones, S_bf[sp_prev][:, c0:], False, True,
                                r=[s_no, s_S[sp_prev]], sgc=True)
                    s_z[zi].w = o_
                    filler(1)
                    ACT(P, wb_[wi][:, c0:], z_ps[zi][:, c0:], AF.Exp, r=[s_z[zi]], w=[s_w[wi]])

                def stage3(tl):
                    n, c0 = tl["n"], tl["c0"]
                    wi, ub, pb, h, kt = n % 3, tl["ub"], tl["pb"], tl["h"], tl["kt"]
                    first = tl["idx"] == 0
                    last = tl["idx"] == tl["Tn"] - 1
                    o_ = MM(P, y_ps[ub][pb:pb + 64, c0:], sbv[:, kt, h * 64:(h + 1) * 64], wb_[wi][:, c0:], first, last,
                            r=[s_w[wi]], w=([s_y[ub]] if first else []), sgc=True)
                    filler(2)
                    if last:
                        s_y[ub].w = o_
                        CP(P, "dve", yT[pb:pb + 64, tl["hp"], tl["b"] * 512:(tl["b"] + 1) * 512],
                           y_ps[ub][pb:pb + 64, :], r=[s_y[ub]])

                NTL = len(tiles)
                for n in range(NTL + 2):
                    if n < NTL:
                        stage1(tiles[n])
                    if 1 <= n <= NTL:
                        stage2(tiles[n - 1])
                    if 2 <= n <= NTL + 1:
                        stage3(tiles[n - 2])
                P.emit()

        if debug == "A2":
            P = Phase(K, "dbgA2")
            fin = [DMA(P, dout("d_yT", [128, 4, S], BF16), yT[:, 0:4, :])]
            finish(P, fin)
            P.emit()
            return nc, dbg

        P = Phase(K, "B")
        with ExitStack() as e4:
            Wr = V(76, [128, 8, 2048], BF16)
            s_Wr = [slots(8) for _ in range(4)]
            wst = [V(108 + 8 * i, [128, 4, 512], F32) for i in range(2)]
            s_wst = slots(2)
            gnbc = V(124, [128, 2, 512], F32)
            retmask = V(128, [128, 128], F32)
            gcb = V(128.5, [128, 4, 64], F32)
            T_ = QKTmp(130, e4, "b")
            qd = [V(146 + i, [128, 512], BF16) for i in range(2)]
            ki = [V(148 + i, [128, 512], BF16) for i in range(2)]
            vb = [V(150 + i, [128, 512], BF16) for i in range(2)]
            gs = [V(152 + i, [128, 512], BF16) for i in range(2)]
            qdT = [V(154 + i, [128, 4, 128], BF16) for i in range(2)]
            kiT = [V(156 + i, [128, 4, 128], BF16) for i in range(2)]
            sm = [V(158 + 2 * i, [128, 2, 4, 128], BF16) for i in range(2)]
            Sst = V(162, [128, 4, 64], F32)
            Stmp = V(163, [128, 4, 64], F32)
            Sbf = [V(164 + 0.5 * i, [128, 4, 64], BF16) for i in range(2)]
            xc = [V(165 + 2 * i, [128, 512], F32) for i in range(2)]
            sq2 = [V(169 + 2 * i, [128, 512], F32) for i in range(2)]
            yg = [V(173 + 2 * i, [128, 512], F32) for i in range(2)]
            ybf = [V(177 + i, [128, 512], BF16) for i in range(2)]
            s1 = [sb("gn_s1%d" % i, [128, 8], F32, e4) for i in range(2)]
            s2 = [sb("gn_s2%d" % i, [128, 8], F32, e4) for i in range(2)]
            s3 = [sb("gn_s3%d" % i, [128, 8], F32, e4) for i in range(2)]
            pj = [ps("pjB%d" % i, [128, 512], F32, e4) for i in range(3)]
            junkB = ps("junkB", [128, 512], F32, e4)
            pj.append(pj[2])
            tpb = ps("tpbB", [128, 4, 128], BF16, e4)
            sTh = [ps("sT%d" % i_, [128, 4, 128], F32, e4) for i_ in range(2)]
            kv_ps = ps("kv_ps", [128, 4, 64], F32, e4)
            s_pj = slots(3); s_pj.append(s_pj[2]); s_tpb = Slot(); s_sTh = slots(2); s_kv = Slot()
            s_qd = slots(2); s_ki = slots(2); s_vb = slots(2); s_gs = slots(2); s_qdT = slots(2); s_kiT = slots(2)
            s_sm = slots(2); s_S = Slot(); s_Stmp = Slot(); s_Sbf = slots(2)
            s_xc = slots(2); s_sq2 = slots(2); s_yg = slots(2); s_ybf = slots(2)
            s_s1 = slots(2); s_s2 = slots(2); s_s3 = slots(2)
            cnt = [0]
            for g in range(4):
                load_w_bf16(P, Wr[:, :, g * 512:(g + 1) * 512], w_in_v[:, :, (3 + g) * 512:(4 + g) * 512], 512, 8,
                            wst, s_wst, s_Wr[g], cnt, scale_col=gmix)
            dl = [DMA(P, gnbc, din("gn_bc")), DMA(P, retmask, din("c_retmask")), DMA(P, gcb, din("c_gcb"))]
            s_cst = Slot(); s_cst.w = None
            o = P.op("dve", lambda e: e.memset(Sst, 0.0), w=[s_S])
            P.op("dve", lambda e: e.memset(Sbf[0], 0.0), w=[s_Sbf[0]])
            def projB(t, groups=(0, 1, 2)):
                for g in groups:
                    for kc in range(KC):
                        o_ = MM(P, pj[g], xT[:, kc, t * 128:(t + 1) * 128], Wr[:, kc, g * 512:(g + 1) * 512],
                                kc == 0, kc == KC - 1, r=[s_Wr[g][kc]], w=([s_pj[g]] if kc == 0 else []))
                    s_pj[g].w = o_

            def fillB(k):
                for _ in range(k):
                    MM(P, junkB, identb, Wr[:, 0, 0:512], True, True, r=[s_Wr[0][0]])

            outp = sTh[0].rearrange("p a b -> p (a b)")
            projB(0)
            for t in range(NT):
                i = t % 2
                post_qk(P, T_, 0, pj[0], s_pj[0], t, 2, True, qdec, qd[i], s_qd[i])
                post_qk(P, T_, 1, pj[1], s_pj[1], t, 3, True, kinv, ki[i], s_ki[i])
                ACT(P, vb[i], pj[2], AF.Copy, r=[s_pj[2]], w=[s_vb[i]], scale=rstd_x[:, t:t + 1])
                fillB(8)
                projB(t, groups=(3,))
                ACT(P, gs[i], pj[3], AF.Silu, r=[s_pj[3]], w=[s_gs[i]], scale=rstd_x[:, t:t + 1])
                fillB(6)
                for (src, s_src, dstT, s_dstT) in [(qd[i], s_qd[i], qdT[i], s_qdT[i]), (ki[i], s_ki[i], kiT[i], s_kiT[i])]:
                    for j in range(4):
                        o = TR(P, tpb[:, j, :], src[:, j * 128:(j + 1) * 128], identb,
                               r=[s_src], x=[getattr(s_src, "w2", None)], w=([s_tpb] if j == 0 else []))
                    s_tpb.w = o
                    CP(P, "dve", dstT, tpb, r=[s_tpb], w=[s_dstT])
                fillB(6)
                for h in range(NH):
                    hp, hh = h // 2, h % 2
                    pb = hh * 64
                    o = MM(P, sTh[hh][:, hp, :], kiT[i][pb:pb + 64, hp, :], qdT[i][pb:pb + 64, hp, :], True, True,
                           r=[s_kiT[i], s_qdT[i]], w=([s_sTh[hh]] if hp == 0 else []))
                    if hp == 3:
                        s_sTh[hh].w = o
                for hh in range(2):
                    o = TT(P, "dve", sm[i][:, hh, :, :], sTh[hh],
                           retmask[:, None, :].broadcast_to([128, 4, 128]), ALU.mult,
                           r=[s_sTh[hh]], w=([s_sm[i]] if hh == 0 else []), x=dl)
                s_sm[i].w = o
                sbf_prev = Sbf[t % 2]
                fillB(6)
                for h in range(NH):
                    hp, pb = h // 2, (h % 2) * 64
                    MM(P, outp[:, h * 64:(h + 1) * 64], sm[i][:, h % 2, h // 2, :], vb[i][:, h * 64:(h + 1) * 64], True, False,
                       r=[s_sm[i], s_vb[i]], w=([s_sTh[0]] if h == 0 else []))
                    o = MM(P, outp[:, h * 64:(h + 1) * 64], qdT[i][pb:pb + 64, hp, :], sbf_prev[pb:pb + 64, hp, :],
                           False, True, r=[s_qdT[i], s_Sbf[t % 2]])
                s_sTh[0].w = o
                for h in range(NH):
                    hp, pb = h // 2, (h % 2) * 64
                    o = MM(P, kv_ps[pb:pb + 64, hp, :], ki[i][:, h * 64:(h + 1) * 64], vb[i][:, h * 64:(h + 1) * 64],
                           True, True, r=[s_ki[i], s_vb[i]], x=[getattr(s_ki[i], "w2", None)],
                           w=([s_kv] if h == 0 else []))
                s_kv.w = o
                TT(P, "dve", Stmp, Sst, kv_ps, ALU.add, r=[s_S, s_kv], w=[s_Stmp])
                TT(P, "dve", Sst, Stmp, gcb, ALU.mult, r=[s_Stmp], w=[s_S], x=dl)
                CP(P, "dve", Sbf[(t + 1) % 2], Sst, r=[s_S], w=[s_Sbf[(t + 1) % 2]])
                if t + 1 < NT:
                    projB(t + 1)
                o3 = outp.rearrange("p (h d) -> p h d", h=8)
                RED(P, s1[i], o3, r=[s_sTh[0]], w=[s_s1[i]])
                TS(P, "dve", s1[i], s1[i], -1.0 / HD, None, ALU.mult, w=[s_s1[i]])
                TT(P, "dve", xc[i].rearrange("p (h d) -> p h d", h=8), o3,
                   s1[i][:, :, None].broadcast_to([128, 8, 64]), ALU.add, r=[s_sTh[0], s_s1[i]], w=[s_xc[i]])
                ACT(P, sq2[i], xc[i], AF.Square, r=[s_xc[i]], w=[s_sq2[i]])
                RED(P, s2[i], sq2[i].rearrange("p (h d) -> p h d", h=8), r=[s_sq2[i]], w=[s_s2[i]])
                ACT(P, s3[i], s2[i], AF.Sqrt, r=[s_s2[i]], w=[s_s3[i]], scale=1.0 / HD, bias=EPS)
                RCP(P, s2[i], s3[i], r=[s_s3[i]], w=[s_s2[i]])
                TT(P, "dve", yg[i].rearrange("p (h d) -> p h d", h=8), xc[i].rearrange("p (h d) -> p h d", h=8),
                   s2[i][:, :, None].broadcast_to([128, 8, 64]), ALU.mult, r=[s_xc[i], s_s2[i]], w=[s_yg[i]])
                TT(P, POOL, yg[i], yg[i], gnbc[:, 0, :], ALU.mult, w=[s_yg[i]], x=dl)
                TT(P, POOL, yg[i], yg[i], gnbc[:, 1, :], ALU.add, w=[s_yg[i]])
                TT(P, POOL, ybf[i], yg[i], gs[i], ALU.mult, r=[s_yg[i], s_gs[i]], w=[s_ybf[i]])
                fillB(12)
                for j in range(4):
                    o = TR(P, tpb[:, j, :], ybf[i][:, j * 128:(j + 1) * 128], identb, r=[s_ybf[i]],
                           w=([s_tpb] if j == 0 else []))
                s_tpb.w = o
                CP(P, "dve", yT[:, 4:8, t * 128:(t + 1) * 128], tpb, r=[s_tpb])
            P.emit()

        if debug in ("B", "Bonly"):
            P = Phase(K, "dbgB")
            if debug == "Bonly" and BSTOP < 99:
                fin = []
            elif debug == "Bonly":
                fin = [DMA(P, dout("d_yTb", [128, 4, S], BF16), yT[:, 4:8, :])]
            else:
                fin = [DMA(P, dout("d_yT", [128, KC, S], BF16), yT)]
            finish(P, fin)
            P.emit()
            return nc, dbg

        xres = V(32, [128, NT, D], F32)
        hnT = V(0, [128, KC, S], BF16)
        P = Phase(K, "C")
        with ExitStack() as e5:
            Wo = V(96, [128, 8, D], BF16)
            s_Wo = slots(8)
            wst = [V(112 + 8 * i, [128, 2, D], F32) for i in range(2)]
            s_wst = slots(2)
            pj = [ps("pjC%d" % i, [128, 512], F32, e5) for i in range(4)]
            s_pj = slots(4)
            s_xr = [slots(2) for _ in range(NT)]
            cnt = [0]
            load_w_bf16(P, Wo, din("w_out").rearrange("(kc p) n -> p kc n", p=128), D, 8, wst, s_wst, s_Wo, cnt)
            x_d = din("x")
            n = 0
            for t in range(NT):
                dx = DMA(P, xres[:, t, :], x_d[t * 128:(t + 1) * 128, :])
                for half in range(2):
                    pi = n % 4
                    n += 1
                    for kc in range(KC):
                        o = MM(P, pj[pi], yT[:, kc, t * 128:(t + 1) * 128], Wo[:, kc, half * 512:(half + 1) * 512],
                               kc == 0, kc == KC - 1, r=[s_Wo[kc]], w=([s_pj[pi]] if kc == 0 else []))
                    s_pj[pi].w = o
                    TT(P, "dve", xres[:, t, half * 512:(half + 1) * 512], pj[pi],
                       xres[:, t, half * 512:(half + 1) * 512], ALU.add, r=[s_pj[pi]], w=[s_xr[t][half]], x=[dx])
            P.emit()

        if debug == "C":
            P = Phase(K, "dbgC")
            fin = [DMA(P, dout("d_x1", [128, NT, D]), xres)]
            finish(P, fin)
            P.emit()
            return nc, dbg

        def norm_transpose(P, t, src, gcol, xs, s_xs, junk, s_junk, ssq, srt_, rst_, tp, s_tp, tcount, evac):
            o = ACT(P, junk, src, AF.Square, w=[s_junk], accum_out=ssq)
            o = ACT(P, srt_, ssq, AF.Sqrt, x=[o], scale=1.0 / D, bias=EPS)
            o = RCP(P, rst_, srt_, x=[o])
            TS(P, "dve", xs, src, rst_, None, ALU.mult, x=[o], w=[s_xs])
            for half in range(2):
                pi = tcount[0] % len(tp)
                tcount[0] += 1
                for j in range(4):
                    kc = half * 4 + j
                    o = TR(P, tp[pi][:, j, :], xs[:, kc * 128:(kc + 1) * 128], ident, r=[s_xs],
                           w=([s_tp[pi]] if j == 0 else []))
                s_tp[pi].w = o
                evac(half * 4, tp[pi], s_tp[pi])

        P = Phase(K, "D")
        with ExitStack() as e6:
            gffn = sb("gffn", [128, KC], F32, e6)
            wr = V(96, [128, KC, NE], F32)
            brb = V(97, [128, NE], F32)
            bdn = V(98, [128, D], F32)[0:NE, :]
            xs = [V(102 + 4 * i, [128, D], F32) for i in range(2)]
            junk = V(110, [128, D], F32)
            hf = [V(114 + 4 * i, [128, KC, 128], F32) for i in range(2)]
            GT = [V(122 + i, [128, 128], F32)[0:NE, :] for i in range(2)]
            s_xs = slots(2); s_junk = Slot(); s_hf = slots(2); s_GT = slots(2)
            ssqA = sb("ssqD", [128, NT], F32, e6)
            srtA = sb("srtD", [128, NT], F32, e6)
            rstA = sb("rstD", [128, NT], F32, e6)
            lg = [sb("lg%d" % i, [128, NE], F32, e6) for i in range(2)]
            top8 = [sb("top8%d" % i, [128, 8], F32, e6) for i in range(2)]
            nmx = [sb("nmx%d" % i, [128, 1], F32, e6) for i in range(2)]
            msk = [sb("msk%d" % i, [128, NE], F32, e6) for i in range(2)]
            ex = [sb("ex%d" % i, [128, NE], F32, e6) for i in range(2)]
            sm_ = [sb("smD%d" % i, [128, 1], F32, e6) for i in range(2)]
            s_lg = slots(2)
            tp = [ps("tpD%d" % i, [128, 4, 128], F32, e6) for i in range(3)]
            s_tp = slots(3)
            lps = [ps("lps%d" % i, [128, NE], F32, e6) for i in range(2)]
            s_lps = slots(2)
            gtp = ps("gtp", [NE, 128], F32, e6)
            s_gtp = Slot()
            bps = [ps("bps%d" % i, [128, 512], F32, e6) for i in range(2)]
            s_bps = slots(2)
            dl = [DMA(P, gffn, din("gffn_col")),
                  DMA(P, wr, din("w_router").rearrange("(kc p) n -> p kc n", p=128)),
                  DMA(P, brb, din("b_router_bc")), DMA(P, bdn, din("b_down"))]
            tcount = [0]
            nbc = [0]

            def frontD(t):
                i = t % 2

                def evac(kc0, tpa, s_tpa, i=i, t=t):
                    o_ = TT(P, "dve", hf[i][:, kc0:kc0 + 4, :], tpa,
                            gffn[:, kc0:kc0 + 4, None].broadcast_to([128, 4, 128]), ALU.mult,
                            r=[s_tpa], w=([s_hf[i]] if kc0 == 0 else []), x=dl)
                    if kc0 == 4:
                        s_hf[i].w = o_
                    o2_ = CP(P, POOL, hnT[:, kc0:kc0 + 4, t * 128:(t + 1) * 128], hf[i][:, kc0:kc0 + 4, :], x=[o_])
                    s_hf[i].r.append(o2_)

                norm_transpose(P, t, xres[:, t, :], None, xs[i], s_xs[i], junk, s_junk, ssqA[:, t:t + 1],
                               srtA[:, t:t + 1], rstA[:, t:t + 1], tp, s_tp, tcount, evac)

            def restD(t):
                i = t % 2
                for kc in range(KC):
                    o = MM(P, lps[i], hf[i][:, kc, :], wr[:, kc, :], kc == 0, kc == KC - 1,
                           r=[s_hf[i]], w=([s_lps[i]] if kc == 0 else []), x=dl)
                s_lps[i].w = o
                o = TT(P, "dve", lg[i], lps[i], brb, ALU.add, r=[s_lps[i]], w=[s_lg[i]], x=dl)
                o = P.op("dve", lambda e, i=i: e.max(out=top8[i], in_=lg[i]), extra=[o])
                o1 = TS(P, "dve", msk[i], lg[i], top8[i][:, 3:4], None, ALU.is_ge, x=[o])
                o2 = TS(P, "dve", nmx[i], top8[i][:, 0:1], -1.0, None, ALU.mult, x=[o])
                o3 = ACT(P, ex[i], lg[i], AF.Exp, x=[o2, o], bias=nmx[i], scale=1.0)
                o4 = TT(P, "dve", ex[i], ex[i], msk[i], ALU.mult, x=[o3, o1])
                o5 = P.op("dve", lambda e, i=i: e.tensor_reduce(out=sm_[i], in_=ex[i], axis=AX.X, op=ALU.add),
                          extra=[o4])
                o6 = RCP(P, sm_[i], sm_[i], x=[o5])
                o7 = TS(P, "dve", G[:, t, :], ex[i], sm_[i], None, ALU.mult, x=[o6])
                s_lg[i].r.extend([o1, o3, o4, o7])
                o = TR(P, gtp, G[:, t, :], ident, x=[o7], w=[s_gtp])
                o = CP(P, "dve", GT[i], gtp, r=[s_gtp], w=[s_GT[i]])
                for half in range(2):
                    bi = nbc[0] % 2
                    nbc[0] += 1
                    MM(P, bps[bi], GT[i], bdn[:, half * 512:(half + 1) * 512], True, True, r=[s_GT[i]],
                       w=[s_bps[bi]], x=dl)
                    TT(P, "dve", xres[:, t, half * 512:(half + 1) * 512], bps[bi],
                       xres[:, t, half * 512:(half + 1) * 512], ALU.add, r=[s_bps[bi]], x=[s_xs[i].w])

            frontD(0)
            for t in range(NT):
                if t + 1 < NT:
                    frontD(t + 1)
                restD(t)
            P.emit()

        if debug == "D":
            P = Phase(K, "dbgD")
            fin = [DMA(P, dout("d_hnT", [128, KC, S], BF16), hnT), DMA(P, dout("d_G", [128, NT, NE]), G),
                   DMA(P, dout("d_x1b", [128, NT, D]), xres)]
            finish(P, fin)
            P.emit()
            return nc, dbg

        P = Phase(K, "M")
        n_exp = NE if debug != "M2" else 2
        with ExitStack() as e7:
            Wu = [V(96 + 16 * i, [128, KC, 1024], BF16) for i in range(2)]
            Wd = [V(128 + 8 * i, [128, 4, D], BF16) for i in range(2)]
            wst = [V(144 + 4 * i, [128, 2, 512], F32) for i in range(4)]
            hact = [V(160 + 4 * i, [128, 4, 512], BF16) for i in range(2)]
            xg = [V(168 + 2 * i, [128, 512], F32) for i in range(2)]
            sg = [V(172 + 2 * i, [128, 512], F32) for i in range(2)]
            tl = [V(176 + 2 * i, [128, 512], F32) for i in range(2)]
            xl = [V(180 + 2 * i, [128, 512], F32) for i in range(2)]
            gsx = [V(184 + 2 * i, [128, 512], F32) for i in range(2)]
            bug = sb("bug", [128, NE, 16], F32, e7)
            s_Wu = [[Slot() for _ in range(16)] for _ in range(2)]
            s_Wd = [slots(4) for _ in range(2)]
            s_wst = slots(4)
            s_hact = [slots(4) for _ in range(2)]
            s_xg = slots(2); s_sg = slots(2); s_tl = slots(2); s_xl = slots(2); s_gsx = slots(2)
            pg = [ps("pg%d" % i, [128, 512], F32, e7) for i in range(2)]
            pl = [ps("pl%d" % i, [128, 512], F32, e7) for i in range(2)]
            py = [ps("py%d" % i, [128, 512], F32, e7) for i in range(3)]
            s_pg = slots(2); s_pl = slots(2); s_py = slots(3)
            s_xr = [slots(2) for _ in range(NT)]
            dbug = DMA(P, bug, din("b_ug_col"))
            wug = din("w_up_gate")
            wdn = din("w_down")
            cnt = [0]
            castn = [0]

            def cast_eng():
                castn[0] += 1
                return "act" if castn[0] % 2 == 0 else "pool"

            def load_unit(e, fh):
                ui = (e * 2 + fh) % 2
                for part in range(2):
                    c0 = part * 1024 + fh * 512
                    src = wug[e].rearrange("(kc p) n -> p kc n", p=128)[:, :, c0:c0 + 512]
                    for k0 in range(0, KC, 2):
                        b = cnt[0] % 4
                        cnt[0] += 1
                        DMA(P, wst[b], src[:, k0:k0 + 2, :], w=[s_wst[b]])
                        for j in range(2):
                            kc = k0 + j
                            eng = cast_eng()
                            dst = Wu[ui][:, kc, part * 512:(part + 1) * 512]
                            if eng == "act":
                                ACT(P, dst, wst[b][:, j, :], AF.Copy, r=[s_wst[b]], w=[s_Wu[ui][kc * 2 + part]])
                            else:
                                CP(P, eng, dst, wst[b][:, j, :], r=[s_wst[b]], w=[s_Wu[ui][kc * 2 + part]])
                srcd = wdn[e].rearrange("(fc p) n -> p fc n", p=128)[:, fh * 4:fh * 4 + 4, :]
                for fc in range(4):
                    b = cnt[0] % 4
                    cnt[0] += 1
                    DMA(P, wst[b].rearrange("p a b -> p (a b)"), srcd[:, fc, :], w=[s_wst[b]])
                    eng = cast_eng()
                    if eng == "act":
                        ACT(P, Wd[ui][:, fc, :], wst[b].rearrange("p a b -> p (a b)"), AF.Copy, r=[s_wst[b]],
                            w=[s_Wd[ui][fc]])
                    else:
                        CP(P, eng, Wd[ui][:, fc, :], wst[b].rearrange("p a b -> p (a b)"), r=[s_wst[b]],
                           w=[s_Wd[ui][fc]])

            ngl = [0]
            nhb = [0]
            npy = [0]

            def up(e, fh, tg):
                ui = (e * 2 + fh) % 2
                hb = nhb[0] % 2
                nhb[0] += 1
                for fc in range(4):
                    gi = ngl[0] % 2
                    ngl[0] += 1
                    for part, pp, s_pp in [(0, pg[gi], s_pg[gi]), (1, pl[gi], s_pl[gi])]:
                        for kc in range(KC):
                            o = MM(P, pp, Wu[ui][:, kc, part * 512 + fc * 128: part * 512 + (fc + 1) * 128],
                                   hnT[:, kc, tg * 512:(tg + 1) * 512], kc == 0, kc == KC - 1,
                                   r=[s_Wu[ui][kc * 2 + part]], w=([s_pp] if kc == 0 else []))
                        s_pp.w = o
                    bg = bug[:, e, fh * 4 + fc: fh * 4 + fc + 1]
                    bl = bug[:, e, 8 + fh * 4 + fc: 8 + fh * 4 + fc + 1]
                    TS(P, "dve", xg[gi], pg[gi], bg, LIMIT, ALU.add, ALU.min, r=[s_pg[gi]], w=[s_xg[gi]], x=[dbug])
                    ACT(P, sg[gi], xg[gi], AF.Sigmoid, r=[s_xg[gi]], w=[s_sg[gi]], scale=ALPHA)
                    TS(P, "dve", tl[gi], pl[gi], bl, LIMIT, ALU.add, ALU.min, r=[s_pl[gi]], w=[s_tl[gi]], x=[dbug])
                    TS(P, "dve", xl[gi], tl[gi], -LIMIT, 1.0, ALU.max, ALU.add, r=[s_tl[gi]], w=[s_xl[gi]])
                    TT(P, POOL, gsx[gi], xg[gi], sg[gi], ALU.mult, r=[s_xg[gi], s_sg[gi]], w=[s_gsx[gi]])
                    TT(P, POOL, hact[hb][:, fc, :], gsx[gi], xl[gi], ALU.mult, r=[s_gsx[gi], s_xl[gi]],
                       w=[s_hact[hb][fc]])
                return hb

            def down(e, fh, tg, hb):
                ui = (e * 2 + fh) % 2
                for tt_ in range(4):
                    t = tg * 4 + tt_
                    for half in range(2):
                        yi = npy[0] % 3
                        npy[0] += 1
                        for fc in range(4):
                            o = MM(P, py[yi], hact[hb][:, fc, tt_ * 128:(tt_ + 1) * 128],
                                   Wd[ui][:, fc, half * 512:(half + 1) * 512], fc == 0, fc == 3,
                                   r=[s_hact[hb][fc], s_Wd[ui][fc]], w=([s_py[yi]] if fc == 0 else []))
                        s_py[yi].w = o
                        xr = xres[:, t, half * 512:(half + 1) * 512]
                        STT(P, xr, py[yi], G[:, t, e:e + 1], xr, ALU.mult, ALU.add, r=[s_py[yi]], w=[s_xr[t][half]])

            units = [(e, fh) for e in range(n_exp) for fh in range(2)]
            load_unit(*units[0])
            pend = None
            for ui_, (e, fh) in enumerate(units):
                for tg in range(4):
                    hb = up(e, fh, tg)
                    if tg == 0 and ui_ + 1 < len(units):
                        pass
                    if pend is not None:
                        down(*pend)
                    pend = (e, fh, tg, hb)
                    if tg == 1 and ui_ + 1 < len(units):
                        load_unit(*units[ui_ + 1])
            down(*pend)
            P.emit()

        if debug in ("M", "M2"):
            P = Phase(K, "dbgM")
            fin = [DMA(P, dout("d_x2", [128, NT, D]), xres)]
            finish(P, fin)
            P.emit()
            return nc, dbg

        P = Phase(K, "L")
        with ExitStack() as e8:
            gple = sb("gple", [128, KC], F32, e8)
            Wpg = V(0, [128, KC, D], BF16)
            Wpp = V(16, [128, 2, D], BF16)
            s_Wpg = slots(8); s_Wpp = slots(2)
            wst = [V(96 + 8 * i, [128, 2, D], F32) for i in range(2)]
            s_wst = slots(2)
            xs = [V(112 + 4 * i, [128, D], F32) for i in range(2)]
            junk = V(120, [128, D], F32)
            x3T = [V(124 + 2 * i, [128, KC, 128], BF16) for i in range(2)]
            pt = [V(128 + i, [128, PLE], F32) for i in range(2)]
            pT = [V(130 + 0.5 * i, [128, 2, 128], BF16) for i in range(2)]
            sgm = [V(132 + 2 * i, [128, 512], F32) for i in range(2)]
            tmp = [V(136 + 2 * i, [128, 512], F32) for i in range(2)]
            ot = [V(140 + 4 * i, [128, D], F32) for i in range(2)]
            s_xs = slots(2); s_junk = Slot(); s_x3T = slots(2); s_pt = slots(2); s_pT = slots(2)
            s_sgm = slots(2); s_tmp = slots(2); s_ot = slots(2)
            ssqA = sb("ssqL", [128, NT], F32, e8)
            srtA = sb("srtL", [128, NT], F32, e8)
            rstA = sb("rstL", [128, NT], F32, e8)
            tp = [ps("tpL%d" % i, [128, 4, 128], F32, e8) for i in range(3)]
            s_tp = slots(3)
            gps = [ps("gps%d" % i, [128, 512], F32, e8) for i in range(2)]
            pps = [ps("pps%d" % i, [128, 512], F32, e8) for i in range(2)]
            s_gps = slots(2); s_pps = slots(2)
            dg = DMA(P, gple, din("gple_col"))
            cnt = [0]
            load_w_bf16(P, Wpg, din("w_ple_gate").rearrange("(kc p) n -> p kc n", p=128), D, 8, wst, s_wst, s_Wpg, cnt,
                        scale_col=None)
            load_w_bf16(P, Wpp, din("w_ple_proj").rearrange("(kc p) n -> p kc n", p=128), D, 2, wst, s_wst, s_Wpp, cnt)
            p_d = din("p")
            tcount = [0]
            fin = []
            nqc = [0]

            def frontL(t):
                i = t % 2

                def evac(kc0, tpa, s_tpa, i=i):
                    o_ = TT(P, "dve", x3T[i][:, kc0:kc0 + 4, :], tpa,
                            gple[:, kc0:kc0 + 4, None].broadcast_to([128, 4, 128]), ALU.mult,
                            r=[s_tpa], w=([s_x3T[i]] if kc0 == 0 else []), x=[dg])
                    if kc0 == 4:
                        s_x3T[i].w = o_

                norm_transpose(P, t, xres[:, t, :], None, xs[i], s_xs[i], junk, s_junk, ssqA[:, t:t + 1],
                               srtA[:, t:t + 1], rstA[:, t:t + 1], tp, s_tp, tcount, evac)
                DMA(P, pt[i], p_d[t * 128:(t + 1) * 128, :], w=[s_pt[i]])
                pi = tcount[0] % 3
                tcount[0] += 1
                for j in range(2):
                    o = TR(P, tp[pi][:, j, :], pt[i][:, j * 128:(j + 1) * 128], ident, r=[s_pt[i]],
                           w=([s_tp[pi]] if j == 0 else []))
                s_tp[pi].w = o
                CP(P, "dve", pT[i], tp[pi][:, 0:2, :], r=[s_tp[pi]], w=[s_pT[i]])

            def backL(t):
                i = t % 2
                for half in range(2):
                    qi = nqc[0] % 2
                    nqc[0] += 1
                    for kc in range(KC):
                        o = MM(P, gps[qi], x3T[i][:, kc, :], Wpg[:, kc, half * 512:(half + 1) * 512], kc == 0,
                               kc == KC - 1, r=[s_x3T[i], s_Wpg[kc]], w=([s_gps[qi]] if kc == 0 else []))
                    s_gps[qi].w = o
                    for c in range(2):
                        o = MM(P, pps[qi], pT[i][:, c, :], Wpp[:, c, half * 512:(half + 1) * 512], c == 0, c == 1,
                               r=[s_pT[i], s_Wpp[c]], w=([s_pps[qi]] if c == 0 else []))
                    s_pps[qi].w = o
                    ACT(P, sgm[qi], gps[qi], AF.Sigmoid, r=[s_gps[qi]], w=[s_sgm[qi]])
                    TT(P, "dve", tmp[qi], pps[qi], sgm[qi], ALU.mult, r=[s_pps[qi], s_sgm[qi]], w=[s_tmp[qi]])
                    TT(P, POOL, ot[i][:, half * 512:(half + 1) * 512], tmp[qi],
                       xres[:, t, half * 512:(half + 1) * 512], ALU.add, r=[s_tmp[qi]],
                       w=([s_ot[i]] if half == 0 else []))
                s_ot[i].w = P.ops["pool"][-1]
                fin.append(DMA(P, out_d[t * 128:(t + 1) * 128, :], ot[i], r=[s_ot[i]]))

            frontL(0)
            for t in range(NT):
                if t + 1 < NT:
                    frontL(t + 1)
                backL(t)
            finish(P, fin)
            P.emit()

    return nc, dbg


def _consts():
    c = {}
    c["c_ident"] = np.eye(128, dtype=np.float32)
    half = HD // 2
    invf = (10000.0 ** (-np.arange(half, dtype=np.float32) / half)).astype(np.float32)
    c["c_invf"] = np.ascontiguousarray(np.broadcast_to(invf[None, :], (128, 32))).astype(np.float32)
    log_g = np.log(1.0 - 2.0 ** (-5.0 - np.arange(NH, dtype=np.float64)))
    idx = np.arange(128, dtype=np.float64)
    c["c_qdec"] = np.exp(log_g[None, :] * (idx[:, None] + 1.0)).astype(np.float32)
    c["c_kinv"] = np.exp(-log_g[None, :] * (idx[:, None] + 1.0)).astype(np.float32)
    gc = np.exp(log_g * 128.0)
    gcb = np.zeros((128, 4, 64), np.float32)
    for h in range(NH):
        gcb[(h % 2) * 64:(h % 2) * 64 + 64, h // 2, :] = gc[h]
    c["c_gcb"] = gcb
    jj, ii = np.meshgrid(np.arange(128), np.arange(128), indexing="ij")
    c["c_retmask"] = (ii >= jj).astype(np.float32)
    c["c_negtri"] = -(jj >= ii).astype(np.float32)
    c["c_negones"] = -np.ones((128, 128), np.float32)
    m = np.zeros((128, 4, 512), np.float32)
    kk = np.arange(128)[:, None]
    qq = np.arange(512)[None, :]
    for i in range(4):
        m[:, i, :] = np.where(kk + 128 * i < qq, 0.0, -30000.0)
    c["c_sbmask"] = m
    return c


def _col(v):
    return np.ascontiguousarray(np.asarray(v, np.float32).reshape(-1, 128).T)


def _bc(v, reps=1):
    v = np.tile(np.asarray(v, np.float32), reps)
    return np.ascontiguousarray(np.broadcast_to(v[None, :], (128, v.shape[0])))


def make_in_maps(x, p, positions, g_mix_norm, w_in, sb_q_gain, sb_k_gain, ret_q_gain, ret_k_gain,
                 ret_gn_gain, ret_gn_bias, w_out, g_ffn_norm, w_router, b_router, w_up_gate, b_up_gate,
                 w_down, b_down, g_ple_norm, w_ple_gate, w_ple_proj):
    f = lambda a: np.ascontiguousarray(np.asarray(a, np.float32))
    shared = dict(_consts())
    shared["gmix_col"] = _col(g_mix_norm[0])
    shared["w_in"] = f(w_in[0])
    shared["gains_bc"] = np.ascontiguousarray(np.stack(
        [_bc(sb_q_gain[0], 8), _bc(sb_k_gain[0], 8), _bc(ret_q_gain[0], 8), _bc(ret_k_gain[0], 8)], axis=1))
    shared["gn_bc"] = np.ascontiguousarray(np.stack([_bc(ret_gn_gain[0]), _bc(ret_gn_bias[0])], axis=1))
    shared["w_out"] = f(w_out[0])
    shared["gffn_col"] = _col(g_ffn_norm[0])
    shared["w_router"] = f(w_router[0])
    shared["b_router_bc"] = _bc(b_router[0])
    shared["w_up_gate"] = f(w_up_gate[0])
    shared["b_ug_col"] = np.ascontiguousarray(
        np.asarray(b_up_gate[0], np.float32).reshape(NE, 16, 128).transpose(2, 0, 1))
    shared["w_down"] = f(w_down[0])
    shared["b_down"] = f(b_down[0])
    shared["gple_col"] = _col(g_ple_norm[0])
    shared["w_ple_gate"] = f(w_ple_gate[0])
    shared["w_ple_proj"] = f(w_ple_proj[0])
    maps = []
    xs = np.asarray(x, np.float32)
    ps_ = np.asarray(p, np.float32)
    pos = np.asarray(positions).astype(np.int32)
    for b in range(8):
        m = dict(shared)
        m["x"] = np.ascontiguousarray(xs[b])
        m["p"] = np.ascontiguousarray(ps_[0, b])
        m["pos_col"] = np.ascontiguousarray(pos[b].reshape(NT, 128).T)
        maps.append(m)
    return maps


def kernel(**inputs):
    nc, info = build_nc()
    maps = make_in_maps(**inputs)
    maps = [{k: v for k, v in m.items() if k in info["_declared"]} for m in maps]
    res = run_bass_kernel_spmd(nc, maps, core_ids=list(range(8)))
    out = np.stack([np.asarray(r["out"], np.float32) for r in res.results], axis=0)
    return out
```

```python
import math
from contextlib import ExitStack

import numpy as np
import concourse.bass as bass
import concourse.mybir as mybir
from concourse.bass_utils import run_bass_kernel_spmd

F32 = mybir.dt.float32
BF16 = mybir.dt.bfloat16
I32 = mybir.dt.int32
AF = mybir.ActivationFunctionType
ALU = mybir.AluOpType
AX = mybir.AxisListType

D = 1024
S = 2048
NT = 16
KC = 8
DIN = 3584
HD = 64
NH = 8
NE = 32
TOPK = 4
PLE = 256
EPS = 1e-6
LIMIT = 7.0
ALPHA = 1.702

ENGS = ["pe", "act", "dve", "pool", "sp"]


class Op:
    __slots__ = ("eng", "fn", "deps", "is_dma", "signal", "count", "sem", "semval", "prewait", "phase")

    def __init__(self, eng, fn, deps, is_dma, phase):
        self.eng = eng
        self.fn = fn
        self.deps = deps
        self.is_dma = is_dma
        self.signal = False
        self.count = None
        self.sem = None
        self.semval = None
        self.prewait = None
        self.phase = phase


class Slot:
    __slots__ = ("w", "r", "w2")

    def __init__(self):
        self.w = None
        self.r = []
        self.w2 = None


class Ctx:
    def __init__(self, nc, es):
        self.nc = nc
        self.esem = {}
        self.ecount = {}
        for e in ["pe", "act", "dve", "pool"]:
            self.esem[e] = es.enter_context(nc.semaphore("es_" + e))
            self.ecount[e] = 0
        self.NDS = 6
        self.dsem = {}
        self.dval = {}
        self.didx = {}
        for e in ["sp", "act", "pool"]:
            self.dsem[e] = [es.enter_context(nc.semaphore("ds_%s%d" % (e, i))) for i in range(self.NDS)]
            self.dval[e] = [0] * self.NDS
            self.didx[e] = 0
        self.phase_id = 0


class Phase:
    def __init__(self, K, name):
        self.K = K
        self.name = name
        self.ops = {e: [] for e in ENGS}
        K.phase_id += 1
        self.id = K.phase_id

    def _add(self, eng, fn, deps, is_dma):
        dl = []
        for d in deps:
            if d is None or d.phase != self.id:
                continue
            if d.eng == "pe" and eng == "pe" and not d.is_dma and not is_dma:
                continue
            if not d.is_dma:
                d.signal = True
            dl.append(d)
        op = Op(eng, fn, dl, is_dma, self.id)
        self.ops[eng].append(op)
        return op

    def op(self, eng, fn, reads=(), writes=(), extra=(), r=None, w=None, x=None):
        reads = r if r is not None else reads
        writes = w if w is not None else writes
        extra = x if x is not None else extra
        deps = list(extra)
        for s in reads:
            deps.append(s.w)
        for s in writes:
            deps.append(s.w)
            deps.extend(s.r)
        o = self._add(eng, fn, deps, False)
        for s in reads:
            s.r.append(o)
        for s in writes:
            s.w = o
            s.r = []
        return o

    def dma(self, eng, fn, reads=(), writes=(), extra=()):
        deps = list(extra)
        for s in reads:
            deps.append(s.w)
        for s in writes:
            deps.append(s.w)
            deps.extend(s.r)
        o = self._add(eng, fn, deps, True)
        for s in reads:
            s.r.append(o)
        for s in writes:
            s.w = o
            s.r = []
        return o

    def emit(self):
        K = self.K
        nc = K.nc
        for e in ENGS:
            for o in self.ops[e]:
                if o.is_dma:
                    j = K.didx[e] % K.NDS
                    K.didx[e] += 1
                    o.sem = K.dsem[e][j]
                    o.prewait = K.dval[e][j]
                    K.dval[e][j] += 16
                    o.semval = K.dval[e][j]
                elif o.signal:
                    K.ecount[e] += 1
                    o.count = K.ecount[e]
        with nc.Block() as block:
            def run(e, engobj):
                waited = {}

                def wait(sem, val):
                    key = id(sem)
                    if waited.get(key, 0) >= val:
                        return
                    engobj.wait_ge(sem, val)
                    waited[key] = val

                for o in self.ops[e]:
                    for d in o.deps:
                        if d.is_dma:
                            wait(d.sem, d.semval)
                        else:
                            wait(K.esem[d.eng], d.count)
                    if o.is_dma:
                        if o.prewait > 0:
                            wait(o.sem, o.prewait)
                        o.fn(engobj).then_inc(o.sem, 16)
                    else:
                        inst = o.fn(engobj)
                        if o.signal:
                            inst.then_inc(K.esem[e], 1)

            if self.ops["pe"]:
                block.tensor(lambda eng: run("pe", eng))
            if self.ops["act"]:
                block.scalar(lambda eng: run("act", eng))
            if self.ops["dve"]:
                block.vector(lambda eng: run("dve", eng))
            if self.ops["pool"]:
                block.gpsimd(lambda eng: run("pool", eng))
            if self.ops["sp"]:
                block.sync(lambda eng: run("sp", eng))


def slots(n):
    return [Slot() for _ in range(n)]


def build_nc(debug=None, POOL="pool", BSTOP=99):
    nc = bass.Bass("TRN2", target_bir_lowering=False)

    SHAPES = {
        "x": ([S, D], F32), "p": ([S, PLE], F32), "pos_col": ([128, NT], I32), "gmix_col": ([128, KC], F32),
        "w_in": ([D, DIN], F32), "gains_bc": ([128, 4, 512], F32), "gn_bc": ([128, 2, 512], F32),
        "w_out": ([D, D], F32), "gffn_col": ([128, KC], F32), "w_router": ([D, NE], F32),
        "b_router_bc": ([128, NE], F32), "w_up_gate": ([NE, D, 2 * D], F32), "b_ug_col": ([128, NE, 16], F32),
        "w_down": ([NE, D, D], F32), "b_down": ([NE, D], F32), "gple_col": ([128, KC], F32),
        "w_ple_gate": ([D, D], F32), "w_ple_proj": ([PLE, D], F32),
        "c_ident": ([128, 128], F32), "c_invf": ([128, 32], F32), "c_qdec": ([128, NH], F32),
        "c_kinv": ([128, NH], F32), "c_gcb": ([128, 4, 64], F32), "c_retmask": ([128, 128], F32),
        "c_negtri": ([128, 128], F32), "c_negones": ([128, 128], F32), "c_sbmask": ([128, 4, 512], F32),
    }
    declared = {}

    def din(name):
        if name not in declared:
            shape, dt = SHAPES[name]
            declared[name] = nc.dram_tensor(name, list(shape), dt, kind="ExternalInput").ap()
        return declared[name]

    out_d = nc.dram_tensor("out", [S, D], F32, kind="ExternalOutput").ap()
    dbg = {"_declared": declared}

    def dout(name, shape, dt=F32):
        t = nc.dram_tensor(name, list(shape), dt, kind="ExternalOutput").ap()
        dbg[name] = t
        return t

    with ExitStack() as es:
        K = Ctx(nc, es)
        ARENA_K = 190
        arena = es.enter_context(nc.sbuf_tensor("arena", [128, ARENA_K * 256], F32))

        def V(off_k, shape, dt):
            n = 1
            for d_ in shape[1:]:
                n *= d_
            off = int(round(off_k * 256))
            if dt == BF16:
                assert n % 2 == 0
                a = arena[:, off:off + n // 2].bitcast(BF16)
                assert off + n // 2 <= ARENA_K * 256
            else:
                a = arena[:, off:off + n]
                if dt != F32:
                    a = a.bitcast(dt)
                assert off + n <= ARENA_K * 256
            if len(shape) == 3:
                a = a.rearrange("p (a b) -> p a b", a=shape[1])
            elif len(shape) == 4:
                a = a.rearrange("p (a b c) -> p a b c", a=shape[1], b=shape[2])
            return a

        def sb(name, shape, dt, stack=es):
            return stack.enter_context(nc.sbuf_tensor(name, list(shape), dt))[:]

        def ps(name, shape, dt, stack=es):
            n = 1
            for d_ in shape[1:]:
                n *= d_
            nbytes = n * (2 if dt == BF16 else 4)
            nbanks = (nbytes + 2047) // 2048
            t = stack.enter_context(nc.psum_tensor(name, [128, 512 * nbanks], F32))[:]
            a = t[:, 0:nbytes // 4]
            if dt == BF16:
                a = a.bitcast(BF16)
            if len(shape) == 3:
                a = a.rearrange("p (a b) -> p a b", a=shape[1])
            if shape[0] != 128:
                a = a[0:shape[0]]
            return a

        def ACT(P, out, in_, func, r=(), w=(), x=(), **kw):
            return P.op("act", lambda e: e.activation(out=out, in_=in_, func=func, **kw), reads=r, writes=w, extra=x)

        def TT(P, eng, out, in0, in1, op, r=(), w=(), x=()):
            return P.op(eng, lambda e: e.tensor_tensor(out=out, in0=in0, in1=in1, op=op), reads=r, writes=w, extra=x)

        def TS(P, eng, out, in0, s1, s2, op0, op1=None, r=(), w=(), x=()):
            if op1 is None:
                return P.op(eng, lambda e: e.tensor_scalar(out=out, in0=in0, scalar1=s1, scalar2=None, op0=op0),
                            reads=r, writes=w, extra=x)
            return P.op(eng, lambda e: e.tensor_scalar(out=out, in0=in0, scalar1=s1, scalar2=s2, op0=op0, op1=op1),
                        reads=r, writes=w, extra=x)

        def STT(P, out, in0, sc, in1, op0, op1, r=(), w=(), x=()):
            return P.op("dve", lambda e: e.scalar_tensor_tensor(out=out, in0=in0, scalar=sc, in1=in1, op0=op0, op1=op1),
                        reads=r, writes=w, extra=x)

        def CP(P, eng, out, in_, r=(), w=(), x=()):
            return P.op(eng, lambda e: e.tensor_copy(out=out, in_=in_), reads=r, writes=w, extra=x)

        def RED(P, out, in_, r=(), w=(), x=()):
            return P.op("dve", lambda e: e.tensor_reduce(out=out, in_=in_, axis=AX.X, op=ALU.add), reads=r, writes=w,
                        extra=x)

        def RCP(P, out, in_, r=(), w=(), x=()):
            return P.op("dve", lambda e: e.reciprocal(out=out, in_=in_), reads=r, writes=w, extra=x)

        def MM(P, out, lhsT, rhs, start, stop, r=(), w=(), x=(), sgc=False):
            return P.op("pe", lambda e: e.matmul(out, lhsT, rhs, start=start, stop=stop, skip_group_check=sgc),
                        reads=r, writes=w, extra=x)

        def TR(P, out, in_, idn, r=(), w=(), x=()):
            return P.op("pe", lambda e: e.transpose(out=out, in_=in_, identity=idn), reads=r, writes=w, extra=x)

        def DMA(P, out, in_, r=(), w=(), x=(), eng="sp"):
            return P.dma(eng, lambda e: e.dma_start(out=out, in_=in_), reads=r, writes=w, extra=x)

        def finish(P, fin):
            P.op("sp", lambda e: e.nop(), extra=fin)

        def load_w_bf16(P, dst, src, ncols, nkc, wst, s_wst, s_dst, cnt, scale_col=None, eng="dve", kstep=None):
            kstep = kstep or wst[0].shape[1]
            for k0 in range(0, nkc, kstep):
                b = cnt[0] % len(wst)
                cnt[0] += 1
                DMA(P, wst[b][:, 0:kstep, 0:ncols], src[:, k0:k0 + kstep, :], w=[s_wst[b]])
                for j in range(kstep):
                    kc = k0 + j
                    if scale_col is not None:
                        TS(P, eng, dst[:, kc, :], wst[b][:, j, 0:ncols], scale_col[:, kc:kc + 1], None, ALU.mult,
                           r=[s_wst[b]], w=[s_dst[kc]])
                    else:
                        CP(P, eng, dst[:, kc, :], wst[b][:, j, 0:ncols], r=[s_wst[b]], w=[s_dst[kc]])

        ident = sb("ident", [128, 128], F32)
        identb = sb("identb", [128, 128], BF16)
        rstd_x = sb("rstd_x", [128, NT], F32)
        qdec = sb("qdec", [128, NH], F32)
        kinv = sb("kinv", [128, NH], F32)
        gmix = sb("gmix", [128, KC], F32)
        G = sb("G", [128, NT, NE], F32)

        yT = V(0, [128, KC, S], BF16)
        xT = V(32, [128, KC, S], BF16)
        gains = V(64, [128, 4, 512], F32)
        cosT = V(72, [128, NT, 32], F32)
        sinT = V(74, [128, NT, 32], F32)

        P = Phase(K, "p1")
        with ExitStack() as e1:
            pos_i = sb("pos_i", [128, NT], I32, e1)
            posf = sb("posf", [128, NT], F32, e1)
            invf = sb("invf", [128, 32], F32, e1)
            ang = V(76, [128, NT, 32], F32)
            ufr = V(78, [128, NT, 32], F32)
            uint_ = V(80, [128, NT, 32], I32)
            uflo = V(82, [128, NT, 32], F32)
            umask = V(84, [128, NT, 32], F32)
            xt = [V(86 + 4 * i, [128, D], F32) for i in range(2)]
            junk = V(94, [128, D], F32)
            ssx = sb("ssx", [128, NT], F32, e1)
            sqx = sb("sqx", [128, NT], F32, e1)
            tp = [ps("tp%d" % i, [128, 4, 128], F32, e1) for i in range(4)]

            cl = []
            for (dst, src) in [(ident, din("c_ident")), (pos_i, din("pos_col")), (invf, din("c_invf")),
                               (gains, din("gains_bc")), (qdec, din("c_qdec")), (kinv, din("c_kinv")),
                               (gmix, din("gmix_col"))]:
                cl.append(DMA(P, dst, src))
            o = CP(P, "dve", identb, ident, x=cl)
            s_ident = Slot(); s_ident.w = cl[0]
            o1 = TS(P, "dve", gains[:, 0, :], gains[:, 0, :], 0.125, None, ALU.mult, x=cl)
            o2 = TS(P, "dve", gains[:, 3, :], gains[:, 3, :], 0.125, None, ALU.mult, x=cl + [o1])
            o = CP(P, "dve", posf, pos_i, x=cl + [o2])
            o = TT(P, "dve", ang, posf[:, :, None].broadcast_to([128, NT, 32]),
                   invf[:, None, :].broadcast_to([128, NT, 32]), ALU.mult, x=[o])
            last = o
            for (dst, off) in [(sinT, 0.5), (cosT, 0.75)]:
                o = TS(P, "dve", ufr, ang, 1.0 / (2 * math.pi), off, ALU.mult, ALU.add, x=[last])
                o = CP(P, "dve", uint_, ufr, x=[o])
                o = CP(P, "dve", uflo, uint_, x=[o])
                o = TT(P, "dve", ufr, ufr, uflo, ALU.subtract, x=[o])
                o = TS(P, "dve", umask, ufr, 0.0, None, ALU.is_lt, x=[o])
                o = TT(P, "dve", ufr, ufr, umask, ALU.add, x=[o])
                o = TS(P, "dve", ufr, ufr, 2 * math.pi, -math.pi, ALU.mult, ALU.add, x=[o])
                o = TS(P, "dve", ufr, ufr, -3.14159, 3.14159, ALU.max, ALU.min, x=[o])
                o = ACT(P, dst, ufr, AF.Sin, x=[o])
                last = o

            s_xt = slots(2)
            s_tp = slots(4)
            s_junk = Slot()
            x_d = din("x")
            fin1 = [last]
            for t in range(NT):
                b = t % 2
                DMA(P, xt[b], x_d[t * 128:(t + 1) * 128, :], w=[s_xt[b]])
                ACT(P, junk, xt[b], AF.Square, r=[s_xt[b]], w=[s_junk], accum_out=ssx[:, t:t + 1])
                for half in range(2):
                    pi = (t * 2 + half) % 4
                    for j in range(4):
                        kc = half * 4 + j
                        o = TR(P, tp[pi][:, j, :], xt[b][:, kc * 128:(kc + 1) * 128], ident,
                               r=[s_xt[b], s_ident], w=([s_tp[pi]] if j == 0 else []))
                    s_tp[pi].w = o
                    fin1.append(CP(P, "dve", xT[:, half * 4:half * 4 + 4, t * 128:(t + 1) * 128], tp[pi],
                                   r=[s_tp[pi]]))
            o = ACT(P, sqx, ssx, AF.Sqrt, x=[s_junk.w], scale=1.0 / D, bias=EPS)
            o = RCP(P, rstd_x, sqx, x=[o])
            P.emit()

        sbqT = V(76, [128, 4, S], BF16)
        sbkT = V(92, [128, 4, S], BF16)
        sbv = V(108, [128, NT, 512], BF16)
        w_in_v = din("w_in").rearrange("(kc p) n -> p kc n", p=128)

        class QKTmp:
            def __init__(self, base_k, stack, tag, n=2):
                self.n = n
                self.qs = [V(base_k + 2 * i, [128, 512], F32) for i in range(n)]
                self.sq = [V(base_k + 2 * n + 2 * i, [128, 512], F32) for i in range(n)]
                self.qn = [V(base_k + 4 * n + 2 * i, [128, 512], F32) for i in range(n)]
                self.ra = [V(base_k + 6 * n + i, [128, 8, 32], F32) for i in range(n)]
                self.rb = [V(base_k + 7 * n + i, [128, 8, 32], F32) for i in range(n)]
                self.ssh = [sb("ssh%s%d" % (tag, i), [128, 8], F32, stack) for i in range(n)]
                self.srt = [sb("srt%s%d" % (tag, i), [128, 8], F32, stack) for i in range(n)]
                self.rsh = [sb("rsh%s%d" % (tag, i), [128, 8], F32, stack) for i in range(n)]
                self.s = {k: slots(n) for k in ["qs", "sq", "qn", "ra", "rb", "ssh", "srt", "rsh"]}
                self.size_k = 8 * n

        def post_qk(P, T_, i, pj_ap, s_pj, t, gi, rope, dec, out_bf, s_out):
            rc = rstd_x[:, t:t + 1]
            s = T_.s
            ACT(P, T_.qs[i], pj_ap, AF.Copy, r=[s_pj], w=[s["qs"][i]], scale=rc)
            ACT(P, T_.sq[i], pj_ap, AF.Square, r=[s_pj], w=[s["sq"][i]], scale=rc)
            RED(P, T_.ssh[i], T_.sq[i].rearrange("p (h d) -> p h d", h=8), r=[s["sq"][i]], w=[s["ssh"][i]])
            ACT(P, T_.srt[i], T_.ssh[i], AF.Sqrt, r=[s["ssh"][i]], w=[s["srt"][i]], scale=1.0 / HD, bias=EPS)
            RCP(P, T_.rsh[i], T_.srt[i], r=[s["srt"][i]], w=[s["rsh"][i]])
            if dec is not None:
                TT(P, "dve", T_.rsh[i], T_.rsh[i], dec, ALU.mult, w=[s["rsh"][i]])
            q3 = T_.qn[i].rearrange("p (h d) -> p h d", h=8)
            TT(P, "dve", q3, T_.qs[i].rearrange("p (h d) -> p h d", h=8),
               T_.rsh[i][:, :, None].broadcast_to([128, 8, 64]), ALU.mult,
               r=[s["qs"][i], s["rsh"][i]], w=[s["qn"][i]])
            if not rope:
                TT(P, "dve", out_bf, T_.qn[i], gains[:, gi, :], ALU.mult, r=[s["qn"][i]], w=[s_out])
                return
            TT(P, "dve", T_.qn[i], T_.qn[i], gains[:, gi, :], ALU.mult, w=[s["qn"][i]])
            prev = [s_out.w, s_out.w2] + list(s_out.r)
            o3 = out_bf.rearrange("p (h d) -> p h d", h=8)
            cb = cosT[:, t:t + 1, :].broadcast_to([128, 8, 32])
            sbb = sinT[:, t:t + 1, :].broadcast_to([128, 8, 32])
            t1 = q3[:, :, 0:32]
            t2 = q3[:, :, 32:64]
            TT(P, POOL, T_.ra[i], t1, cb, ALU.mult, r=[s["qn"][i]], w=[s["ra"][i]])
            TT(P, POOL, T_.rb[i], t2, sbb, ALU.mult, r=[s["qn"][i]], w=[s["rb"][i]])
            TT(P, POOL, o3[:, :, 0:32], T_.ra[i], T_.rb[i], ALU.subtract, r=[s["ra"][i], s["rb"][i]], w=[s_out])
            TT(P, "dve", T_.ra[i], t2, cb, ALU.mult, r=[s["qn"][i]], w=[s["ra"][i]])
            TT(P, "dve", T_.rb[i], t1, sbb, ALU.mult, r=[s["qn"][i]], w=[s["rb"][i]])
            o = TT(P, "dve", o3[:, :, 32:64], T_.ra[i], T_.rb[i], ALU.add, r=[s["ra"][i], s["rb"][i]], x=prev)
            s_out.r.append(o)
            s_out.w2 = o

        P = Phase(K, "A1")
        with ExitStack() as e2:
          if debug != "Bonly":
                wst = [V(124 + 8 * i, [128, 4, 512], F32) for i in range(2)]
                s_wst = slots(2)
                Wg = [V(140 + 8 * i, [128, 8, 512], BF16) for i in range(2)]
                s_Wg = [slots(8) for _ in range(2)]
                T_ = QKTmp(156, e2, "a")
                qbf = [V(172 + i, [128, 512], BF16) for i in range(2)]
                s_qbf = slots(2)
                pj = [ps("pjA%d" % i, [128, 512], F32, e2) for i in range(2)]
                s_pj = slots(2)
                tpb = [ps("tpbA%d" % i, [128, 4, 128], BF16, e2) for i in range(2)]
                s_tpb = slots(2)
                cnt = [0]
                n_i = 0
                for cg in range(3):
                    wb = cg % 2
                    load_w_bf16(P, Wg[wb], w_in_v[:, :, cg * 512:(cg + 1) * 512], 512, 8, wst, s_wst, s_Wg[wb], cnt,
                                scale_col=gmix)
                    def projA(t, wb=wb):
                        pb_ = t % 2
                        for kc in range(KC):
                            o_ = MM(P, pj[pb_], xT[:, kc, t * 128:(t + 1) * 128], Wg[wb][:, kc, :], kc == 0, kc == KC - 1,
                                    r=[s_Wg[wb][kc]], w=([s_pj[pb_]] if kc == 0 else []))
                        s_pj[pb_].w = o_

                    projA(0)
                    for t in range(NT):
                        pb = t % 2
                        if t + 1 < NT:
                            projA(t + 1)
                        if cg < 2:
                            i = n_i % 2
                            n_i += 1
                            post_qk(P, T_, i, pj[pb], s_pj[pb], t, cg, False, None, qbf[i], s_qbf[i])
                            for j in range(4):
                                o = TR(P, tpb[i][:, j, :], qbf[i][:, j * 128:(j + 1) * 128], identb,
                                       r=[s_qbf[i]], w=([s_tpb[i]] if j == 0 else []))
                            s_tpb[i].w = o
                            dst = (sbqT if cg == 0 else sbkT)[:, :, t * 128:(t + 1) * 128]
                            CP(P, "dve", dst, tpb[i], r=[s_tpb[i]])
                        else:
                            ACT(P, sbv[:, t, :], pj[pb], AF.Copy, r=[s_pj[pb]], scale=rstd_x[:, t:t + 1])
                P.emit()

        if debug == "A1":
            P = Phase(K, "dbgA1")
            fin = [DMA(P, dout("d_sbqT", [128, 4, S], BF16), sbqT), DMA(P, dout("d_sbkT", [128, 4, S], BF16), sbkT),
                   DMA(P, dout("d_sbv", [128, NT, 512], BF16), sbv)]
            finish(P, fin)
            P.emit()
            return nc, dbg

        P = Phase(K, "A2")
        with ExitStack() as e3:
          if debug != "Bonly":
                NSP = 4
                sp_bf = [V(124 + i, [128, 512], BF16) for i in range(NSP)]
                S_bf = [V(128 + i, [128, 512], BF16) for i in range(NSP)]
                acc = V(132, [128, 512], F32)
                wb_ = [V(134 + i, [128, 512], BF16) for i in range(3)]
                etmp = [V(137 + 2 * i, [128, 512], F32) for i in range(2)]
                sbmask = V(141, [128, 4, 512], BF16)
                negtri = V(145, [128, 128], BF16)
                negones = V(145.25, [128, 128], BF16)
                cst = V(146, [128, 4, 512], F32)
                cst2 = V(154, [128, 2, 128], F32)
                NZ = 5
                z_ps = [ps("z_ps%d" % i, [128, 512], F32, e3) for i in range(NZ)]
                y_ps = [ps("y_ps%d" % i, [128, 512], F32, e3) for i in range(2)]
                junk_ps = ps("junk_ps", [128, 512], F32, e3)
                s_z = slots(NZ); s_y = slots(2); s_w = slots(3); s_et = slots(2)
                s_sp = slots(NSP); s_S = slots(NSP); s_acc = Slot()
                d1 = DMA(P, cst, din("c_sbmask"))
                d2 = DMA(P, cst2[:, 0, :], din("c_negtri"))
                d3 = DMA(P, cst2[:, 1, :], din("c_negones"))
                s_cm = Slot(); s_cm.w = CP(P, "dve", sbmask, cst, x=[d1])
                s_nt = Slot(); s_nt.w = CP(P, "dve", negtri, cst2[:, 0, :], x=[d2])
                s_no = Slot(); s_no.w = CP(P, "dve", negones, cst2[:, 1, :], x=[d3])
                tiles = []
                u = 0
                for hp in range(4):
                    for hh in range(2):
                        for b in range(4):
                            Tn = 4 * b + 4
                            for idx in range(Tn):
                                kt = Tn - 1 - idx
                                tiles.append(dict(hp=hp, hh=hh, h=2 * hp + hh, pb=hh * 64, b=b, Tn=Tn, idx=idx, kt=kt,
                                                  band=(kt >= 4 * b), mi=kt - 4 * b, ub=u % 2, n=len(tiles),
                                                  c0=(128 * (kt - 4 * b) if kt >= 4 * b else 0)))
                            u += 1

                def filler(k=1):
                    for _ in range(k):
                        MM(P, junk_ps, negones, sbmask[:, 0, :], True, True, r=[s_no, s_cm])

                def qk(tl, dst, s_dst, last):
                    pb, hp, b, kt, c0 = tl["pb"], tl["hp"], tl["b"], tl["kt"], tl["c0"]
                    o_ = MM(P, dst[:, c0:], sbkT[pb:pb + 64, hp, kt * 128:(kt + 1) * 128],
                            sbqT[pb:pb + 64, hp, b * 512 + c0:(b + 1) * 512], True, last and not tl["band"], w=[s_dst])
                    if tl["band"]:
                        o_ = MM(P, dst[:, c0:], identb, sbmask[:, tl["mi"], c0:], False, last, r=[s_cm])
                    return o_

                def stage1(tl):
                    n, c0 = tl["n"], tl["c0"]
                    zi, ei, si = n % NZ, n % 2, n % NSP
                    o_ = qk(tl, z_ps[zi], s_z[zi], True)
                    s_z[zi].w = o_
                    filler(1)
                    ACT(P, etmp[ei][:, c0:], z_ps[zi][:, c0:], AF.Exp, r=[s_z[zi]], w=[s_et[ei]])
                    ACT(P, sp_bf[si][:, c0:], etmp[ei][:, c0:], AF.Ln, r=[s_et[ei]], w=[s_sp[si]], bias=1.0)
                    if tl["idx"] < tl["Tn"] - 1:
                        if tl["idx"] == 0:
                            P.op("dve", lambda e, c0=c0: e.memset(acc[:, 0:c0], 0.0), w=[s_acc])
                            CP(P, "dve", acc[:, c0:], sp_bf[si][:, c0:], r=[s_sp[si]], w=[s_acc])
                        else:
                            TT(P, "dve", acc[:, c0:], acc[:, c0:], sp_bf[si][:, c0:], ALU.add, r=[s_sp[si]], w=[s_acc])
                        CP(P, "dve", S_bf[si], acc, r=[s_acc], w=[s_S[si]])

                def stage2(tl):
                    n, c0 = tl["n"], tl["c0"]
                    zi, wi, si = n % NZ, n % 3, n % NSP
                    first = tl["idx"] == 0
                    o_ = MM(P, z_ps[zi][:, c0:], negtri, sp_bf[si][:, c0:], False, first, r=[s_nt, s_sp[si]],
                            w=[s_z[zi]], sgc=True)
                    if not first:
                        sp_prev = (n - 1) % NSP
                        o_ = MM(P, z_ps[zi][:, c0:], negones, S_bf[sp_prev][:, c0:], False, True,
                                r=[s_no, s_S[sp_prev]], sgc=True)
                    s_z[zi].w = o_
                    filler(1)
                    ACT(P, wb_[wi][:, c0:], z_ps[zi][:, c0:], AF.Exp, r=[s_z[zi]], w=[s_w[wi]])

                def stage3(tl):
                    n, c0 = tl["n"], tl["c0"]
                    wi, ub, pb, h, kt = n % 3, tl["ub"], tl["pb"], tl["h"], tl["kt"]
                    first = tl["idx"] == 0
                    last = tl["idx"] == tl["Tn"] - 1
                    o_ = MM(P, y_ps[ub][pb:pb + 64, c0:], sbv[:, kt, h * 64:(h + 1) * 64], wb_[wi][:, c0:], first, last,
                            r=[s_w[wi]], w=([s_y[ub]] if first else []), sgc=True)
                    filler(2)
                    if last:
                        s_y[ub].w = o_
                        CP(P, "dve", yT[pb:pb + 64, tl["hp"], tl["b"] * 512:(tl["b"] + 1) * 512],
                           y_ps[ub][pb:pb + 64, :], r=[s_y[ub]])

                NTL = len(tiles)
                for n in range(NTL + 2):
                    if n < NTL:
                        stage1(tiles[n])
                    if 1 <= n <= NTL:
                        stage2(tiles[n - 1])
                    if 2 <= n <= NTL + 1:
                        stage3(tiles[n - 2])
                P.emit()

        if debug == "A2":
            P = Phase(K, "dbgA2")
            fin = [DMA(P, dout("d_yT", [128, 4, S], BF16), yT[:, 0:4, :])]
            finish(P, fin)
            P.emit()
            return nc, dbg

        P = Phase(K, "B")
        with ExitStack() as e4:
            Wr = V(76, [128, 8, 2048], BF16)
            s_Wr = [slots(8) for _ in range(4)]
            wst = [V(108 + 8 * i, [128, 4, 512], F32) for i in range(2)]
            s_wst = slots(2)
            gnbc = V(124, [128, 2, 512], F32)
            retmask = V(128, [128, 128], F32)
            gcb = V(128.5, [128, 4, 64], F32)
            T_ = QKTmp(130, e4, "b")
            qd = [V(146 + i, [128, 512], BF16) for i in range(2)]
            ki = [V(148 + i, [128, 512], BF16) for i in range(2)]
            vb = [V(150 + i, [128, 512], BF16) for i in range(2)]
            gs = [V(152 + i, [128, 512], BF16) for i in range(2)]
            qdT = [V(154 + i, [128, 4, 128], BF16) for i in range(2)]
            kiT = [V(156 + i, [128, 4, 128], BF16) for i in range(2)]
            sm = [V(158 + 2 * i, [128, 2, 4, 128], BF16) for i in range(2)]
            Sst = V(162, [128, 4, 64], F32)
            Stmp = V(163, [128, 4, 64], F32)
            Sbf = [V(164 + 0.5 * i, [128, 4, 64], BF16) for i in range(2)]
            xc = [V(165 + 2 * i, [128, 512], F32) for i in range(2)]
            sq2 = [V(169 + 2 * i, [128, 512], F32) for i in range(2)]
            yg = [V(173 + 2 * i, [128, 512], F32) for i in range(2)]
            ybf = [V(177 + i, [128, 512], BF16) for i in range(2)]
            s1 = [sb("gn_s1%d" % i, [128, 8], F32, e4) for i in range(2)]
            s2 = [sb("gn_s2%d" % i, [128, 8], F32, e4) for i in range(2)]
            s3 = [sb("gn_s3%d" % i, [128, 8], F32, e4) for i in range(2)]
            pj = [ps("pjB%d" % i, [128, 512], F32, e4) for i in range(3)]
            junkB = ps("junkB", [128, 512], F32, e4)
            pj.append(pj[2])
            tpb = ps("tpbB", [128, 4, 128], BF16, e4)
            sTh = [ps("sT%d" % i_, [128, 4, 128], F32, e4) for i_ in range(2)]
            kv_ps = ps("kv_ps", [128, 4, 64], F32, e4)
            s_pj = slots(3); s_pj.append(s_pj[2]); s_tpb = Slot(); s_sTh = slots(2); s_kv = Slot()
            s_qd = slots(2); s_ki = slots(2); s_vb = slots(2); s_gs = slots(2); s_qdT = slots(2); s_kiT = slots(2)
            s_sm = slots(2); s_S = Slot(); s_Stmp = Slot(); s_Sbf = slots(2)
            s_xc = slots(2); s_sq2 = slots(2); s_yg = slots(2); s_ybf = slots(2)
            s_s1 = slots(2); s_s2 = slots(2); s_s3 = slots(2)
            cnt = [0]
            for g in range(4):
                load_w_bf16(P, Wr[:, :, g * 512:(g + 1) * 512], w_in_v[:, :, (3 + g) * 512:(4 + g) * 512], 512, 8,
                            wst, s_wst, s_Wr[g], cnt, scale_col=gmix)
            dl = [DMA(P, gnbc, din("gn_bc")), DMA(P, retmask, din("c_retmask")), DMA(P, gcb, din("c_gcb"))]
            s_cst = Slot(); s_cst.w = None
            o = P.op("dve", lambda e: e.memset(Sst, 0.0), w=[s_S])
            P.op("dve", lambda e: e.memset(Sbf[0], 0.0), w=[s_Sbf[0]])
            def projB(t, groups=(0, 1, 2)):
                for g in groups:
                    for kc in range(KC):
                        o_ = MM(P, pj[g], xT[:, kc, t * 128:(t + 1) * 128], Wr[:, kc, g * 512:(g + 1) * 512],
                                kc == 0, kc == KC - 1, r=[s_Wr[g][kc]], w=([s_pj[g]] if kc == 0 else []))
                    s_pj[g].w = o_

            def fillB(k):
                for _ in range(k):
                    MM(P, junkB, identb, Wr[:, 0, 0:512], True, True, r=[s_Wr[0][0]])

            outp = sTh[0].rearrange("p a b -> p (a b)")
            projB(0)
            for t in range(NT):
                i = t % 2
                post_qk(P, T_, 0, pj[0], s_pj[0], t, 2, True, qdec, qd[i], s_qd[i])
                post_qk(P, T_, 1, pj[1], s_pj[1], t, 3, True, kinv, ki[i], s_ki[i])
                ACT(P, vb[i], pj[2], AF.Copy, r=[s_pj[2]], w=[s_vb[i]], scale=rstd_x[:, t:t + 1])
                fillB(8)
                projB(t, groups=(3,))
                ACT(P, gs[i], pj[3], AF.Silu, r=[s_pj[3]], w=[s_gs[i]], scale=rstd_x[:, t:t + 1])
                fillB(6)
                for (src, s_src, dstT, s_dstT) in [(qd[i], s_qd[i], qdT[i], s_qdT[i]), (ki[i], s_ki[i], kiT[i], s_kiT[i])]:
                    for j in range(4):
                        o = TR(P, tpb[:, j, :], src[:, j * 128:(j + 1) * 128], identb,
                               r=[s_src], x=[getattr(s_src, "w2", None)], w=([s_tpb] if j == 0 else []))
                    s_tpb.w = o
                    CP(P, "dve", dstT, tpb, r=[s_tpb], w=[s_dstT])
                fillB(6)
                for h in range(NH):
                    hp, hh = h // 2, h % 2
                    pb = hh * 64
                    o = MM(P, sTh[hh][:, hp, :], kiT[i][pb:pb + 64, hp, :], qdT[i][pb:pb + 64, hp, :], True, True,
                           r=[s_kiT[i], s_qdT[i]], w=([s_sTh[hh]] if hp == 0 else []))
                    if hp == 3:
                        s_sTh[hh].w = o
                for hh in range(2):
                    o = TT(P, "dve", sm[i][:, hh, :, :], sTh[hh],
                           retmask[:, None, :].broadcast_to([128, 4, 128]), ALU.mult,
                           r=[s_sTh[hh]], w=([s_sm[i]] if hh == 0 else []), x=dl)
                s_sm[i].w = o
                sbf_prev = Sbf[t % 2]
                fillB(6)
                for h in range(NH):
                    hp, pb = h // 2, (h % 2) * 64
                    MM(P, outp[:, h * 64:(h + 1) * 64], sm[i][:, h % 2, h // 2, :], vb[i][:, h * 64:(h + 1) * 64], True, False,
                       r=[s_sm[i], s_vb[i]], w=([s_sTh[0]] if h == 0 else []))
                    o = MM(P, outp[:, h * 64:(h + 1) * 64], qdT[i][pb:pb + 64, hp, :], sbf_prev[pb:pb + 64, hp, :],
                           False, True, r=[s_qdT[i], s_Sbf[t % 2]])
                s_sTh[0].w = o
                for h in range(NH):
                    hp, pb = h // 2, (h % 2) * 64
                    o = MM(P, kv_ps[pb:pb + 64, hp, :], ki[i][:, h * 64:(h + 1) * 64], vb[i][:, h * 64:(h + 1) * 64],
                           True, True, r=[s_ki[i], s_vb[i]], x=[getattr(s_ki[i], "w2", None)],
                           w=([s_kv] if h == 0 else []))
                s_kv.w = o
                TT(P, "dve", Stmp, Sst, kv_ps, ALU.add, r=[s_S, s_kv], w=[s_Stmp])
                TT(P, "dve", Sst, Stmp, gcb, ALU.mult, r=[s_Stmp], w=[s_S], x=dl)
                CP(P, "dve", Sbf[(t + 1) % 2], Sst, r=[s_S], w=[s_Sbf[(t + 1) % 2]])
                if t + 1 < NT:
                    projB(t + 1)
                o3 = outp.rearrange("p (h d) -> p h d", h=8)
                RED(P, s1[i], o3, r=[s_sTh[0]], w=[s_s1[i]])
                TS(P, "dve", s1[i], s1[i], -1.0 / HD, None, ALU.mult, w=[s_s1[i]])
                TT(P, "dve", xc[i].rearrange("p (h d) -> p h d", h=8), o3,
                   s1[i][:, :, None].broadcast_to([128, 8, 64]), ALU.add, r=[s_sTh[0], s_s1[i]], w=[s_xc[i]])
                ACT(P, sq2[i], xc[i], AF.Square, r=[s_xc[i]], w=[s_sq2[i]])
                RED(P, s2[i], sq2[i].rearrange("p (h d) -> p h d", h=8), r=[s_sq2[i]], w=[s_s2[i]])
                ACT(P, s3[i], s2[i], AF.Sqrt, r=[s_s2[i]], w=[s_s3[i]], scale=1.0 / HD, bias=EPS)
                RCP(P, s2[i], s3[i], r=[s_s3[i]], w=[s_s2[i]])
                TT(P, "dve", yg[i].rearrange("p (h d) -> p h d", h=8), xc[i].rearrange("p (h d) -> p h d", h=8),
                   s2[i][:, :, None].broadcast_to([128, 8, 64]), ALU.mult, r=[s_xc[i], s_s2[i]], w=[s_yg[i]])
                TT(P, POOL, yg[i], yg[i], gnbc[:, 0, :], ALU.mult, w=[s_yg[i]], x=dl)
                TT(P, POOL, yg[i], yg[i], gnbc[:, 1, :], ALU.add, w=[s_yg[i]])
                TT(P, POOL, ybf[i], yg[i], gs[i], ALU.mult, r=[s_yg[i], s_gs[i]], w=[s_ybf[i]])
                fillB(12)
                for j in range(4):
                    o = TR(P, tpb[:, j, :], ybf[i][:, j * 128:(j + 1) * 128], identb, r=[s_ybf[i]],
                           w=([s_tpb] if j == 0 else []))
                s_tpb.w = o
                CP(P, "dve", yT[:, 4:8, t * 128:(t + 1) * 128], tpb, r=[s_tpb])
            P.emit()

        if debug in ("B", "Bonly"):
            P = Phase(K, "dbgB")
            if debug == "Bonly" and BSTOP < 99:
                fin = []
            elif debug == "Bonly":
                fin = [DMA(P, dout("d_yTb", [128, 4, S], BF16), yT[:, 4:8, :])]
            else:
                fin = [DMA(P, dout("d_yT", [128, KC, S], BF16), yT)]
            finish(P, fin)
            P.emit()
            return nc, dbg

        xres = V(32, [128, NT, D], F32)
        hnT = V(0, [128, KC, S], BF16)
        P = Phase(K, "C")
        with ExitStack() as e5:
            Wo = V(96, [128, 8, D], BF16)
            s_Wo = slots(8)
            wst = [V(112 + 8 * i, [128, 2, D], F32) for i in range(2)]
            s_wst = slots(2)
            pj = [ps("pjC%d" % i, [128, 512], F32, e5) for i in range(4)]
            s_pj = slots(4)
            s_xr = [slots(2) for _ in range(NT)]
            cnt = [0]
            load_w_bf16(P, Wo, din("w_out").rearrange("(kc p) n -> p kc n", p=128), D, 8, wst, s_wst, s_Wo, cnt)
            x_d = din("x")
            n = 0
            for t in range(NT):
                dx = DMA(P, xres[:, t, :], x_d[t * 128:(t + 1) * 128, :])
                for half in range(2):
                    pi = n % 4
                    n += 1
                    for kc in range(KC):
                        o = MM(P, pj[pi], yT[:, kc, t * 128:(t + 1) * 128], Wo[:, kc, half * 512:(half + 1) * 512],
                               kc == 0, kc == KC - 1, r=[s_Wo[kc]], w=([s_pj[pi]] if kc == 0 else []))
                    s_pj[pi].w = o
                    TT(P, "dve", xres[:, t, half * 512:(half + 1) * 512], pj[pi],
                       xres[:, t, half * 512:(half + 1) * 512], ALU.add, r=[s_pj[pi]], w=[s_xr[t][half]], x=[dx])
            P.emit()

        if debug == "C":
            P = Phase(K, "dbgC")
            fin = [DMA(P, dout("d_x1", [128, NT, D]), xres)]
            finish(P, fin)
            P.emit()
            return nc, dbg

        def norm_transpose(P, t, src, gcol, xs, s_xs, junk, s_junk, ssq, srt_, rst_, tp, s_tp, tcount, evac):
            o = ACT(P, junk, src, AF.Square, w=[s_junk], accum_out=ssq)
            o = ACT(P, srt_, ssq, AF.Sqrt, x=[o], scale=1.0 / D, bias=EPS)
            o = RCP(P, rst_, srt_, x=[o])
            TS(P, "dve", xs, src, rst_, None, ALU.mult, x=[o], w=[s_xs])
            for half in range(2):
                pi = tcount[0] % len(tp)
                tcount[0] += 1
                for j in range(4):
                    kc = half * 4 + j
                    o = TR(P, tp[pi][:, j, :], xs[:, kc * 128:(kc + 1) * 128], ident, r=[s_xs],
                           w=([s_tp[pi]] if j == 0 else []))
                s_tp[pi].w = o
                evac(half * 4, tp[pi], s_tp[pi])

        P = Phase(K, "D")
        with ExitStack() as e6:
            gffn = sb("gffn", [128, KC], F32, e6)
            wr = V(96, [128, KC, NE], F32)
            brb = V(97, [128, NE], F32)
            bdn = V(98, [128, D], F32)[0:NE, :]
            xs = [V(102 + 4 * i, [128, D], F32) for i in range(2)]
            junk = V(110, [128, D], F32)
            hf = [V(114 + 4 * i, [128, KC, 128], F32) for i in range(2)]
            GT = [V(122 + i, [128, 128], F32)[0:NE, :] for i in range(2)]
            s_xs = slots(2); s_junk = Slot(); s_hf = slots(2); s_GT = slots(2)
            ssqA = sb("ssqD", [128, NT], F32, e6)
            srtA = sb("srtD", [128, NT], F32, e6)
            rstA = sb("rstD", [128, NT], F32, e6)
            lg = [sb("lg%d" % i, [128, NE], F32, e6) for i in range(2)]
            top8 = [sb("top8%d" % i, [128, 8], F32, e6) for i in range(2)]
            nmx = [sb("nmx%d" % i, [128, 1], F32, e6) for i in range(2)]
            msk = [sb("msk%d" % i, [128, NE], F32, e6) for i in range(2)]
            ex = [sb("ex%d" % i, [128, NE], F32, e6) for i in range(2)]
            sm_ = [sb("smD%d" % i, [128, 1], F32, e6) for i in range(2)]
            s_lg = slots(2)
            tp = [ps("tpD%d" % i, [128, 4, 128], F32, e6) for i in range(2)]
            s_tp = slots(2)
            junkD = ps("junkD", [128, 512], F32, e6)

            def fillD(k):
                for _ in range(k):
                    MM(P, junkD[:, 0:128], ident, ident, True, True)
            lps = [ps("lps%d" % i, [128, NE], F32, e6) for i in range(2)]
            s_lps = slots(2)
            gtp = ps("gtp", [NE, 128], F32, e6)
            s_gtp = Slot()
            bps = [ps("bps%d" % i, [128, 512], F32, e6) for i in range(2)]
            s_bps = slots(2)
            dl = [DMA(P, gffn, din("gffn_col")),
                  DMA(P, wr, din("w_router").rearrange("(kc p) n -> p kc n", p=128)),
                  DMA(P, brb, din("b_router_bc")), DMA(P, bdn, din("b_down"))]
            tcount = [0]
            nbc = [0]

            def frontD(t):
                i = t % 2

                def evac(kc0, tpa, s_tpa, i=i, t=t):
                    o_ = TT(P, "dve", hf[i][:, kc0:kc0 + 4, :], tpa,
                            gffn[:, kc0:kc0 + 4, None].broadcast_to([128, 4, 128]), ALU.mult,
                            r=[s_tpa], w=([s_hf[i]] if kc0 == 0 else []), x=dl)
                    if kc0 == 4:
                        s_hf[i].w = o_
                    o2_ = CP(P, POOL, hnT[:, kc0:kc0 + 4, t * 128:(t + 1) * 128], hf[i][:, kc0:kc0 + 4, :], x=[o_])
                    s_hf[i].r.append(o2_)

                fillD(4)
                norm_transpose(P, t, xres[:, t, :], None, xs[i], s_xs[i], junk, s_junk, ssqA[:, t:t + 1],
                               srtA[:, t:t + 1], rstA[:, t:t + 1], tp, s_tp, tcount, evac)

            def restD(t):
                i = t % 2
                fillD(4)
                for kc in range(KC):
                    o = MM(P, lps[i], hf[i][:, kc, :], wr[:, kc, :], kc == 0, kc == KC - 1,
                           r=[s_hf[i]], w=([s_lps[i]] if kc == 0 else []), x=dl)
                s_lps[i].w = o
                o = TT(P, "dve", lg[i], lps[i], brb, ALU.add, r=[s_lps[i]], w=[s_lg[i]], x=dl)
                o = P.op("dve", lambda e, i=i: e.max(out=top8[i], in_=lg[i]), extra=[o])
                o1 = TS(P, "dve", msk[i], lg[i], top8[i][:, 3:4], None, ALU.is_ge, x=[o])
                o2 = TS(P, "dve", nmx[i], top8[i][:, 0:1], -1.0, None, ALU.mult, x=[o])
                o3 = ACT(P, ex[i], lg[i], AF.Exp, x=[o2, o], bias=nmx[i], scale=1.0)
                o4 = TT(P, "dve", ex[i], ex[i], msk[i], ALU.mult, x=[o3, o1])
                o5 = P.op("dve", lambda e, i=i: e.tensor_reduce(out=sm_[i], in_=ex[i], axis=AX.X, op=ALU.add),
                          extra=[o4])
                o6 = RCP(P, sm_[i], sm_[i], x=[o5])
                o7 = TS(P, "dve", G[:, t, :], ex[i], sm_[i], None, ALU.mult, x=[o6])
                s_lg[i].r.extend([o1, o3, o4, o7])
                fillD(4)
                o = TR(P, gtp, G[:, t, :], ident, x=[o7], w=[s_gtp])
                o = CP(P, "dve", GT[i], gtp, r=[s_gtp], w=[s_GT[i]])
                fillD(3)
                for half in range(2):
                    bi = nbc[0] % 2
                    nbc[0] += 1
                    MM(P, bps[bi], GT[i], bdn[:, half * 512:(half + 1) * 512], True, True, r=[s_GT[i]],
                       w=[s_bps[bi]], x=dl)
                    TT(P, "dve", xres[:, t, half * 512:(half + 1) * 512], bps[bi],
                       xres[:, t, half * 512:(half + 1) * 512], ALU.add, r=[s_bps[bi]], x=[s_xs[i].w])

            frontD(0)
            for t in range(NT):
                if t + 1 < NT:
                    frontD(t + 1)
                restD(t)
            P.emit()

        if debug == "D":
            P = Phase(K, "dbgD")
            fin = [DMA(P, dout("d_hnT", [128, KC, S], BF16), hnT), DMA(P, dout("d_G", [128, NT, NE]), G),
                   DMA(P, dout("d_x1b", [128, NT, D]), xres)]
            finish(P, fin)
            P.emit()
            return nc, dbg

        P = Phase(K, "M")
        n_exp = NE if debug != "M2" else 2
        with ExitStack() as e7:
            Wu = [V(96 + 16 * i, [128, KC, 1024], BF16) for i in range(2)]
            Wd = [V(128 + 8 * i, [128, 4, D], BF16) for i in range(2)]
            wst = [V(144 + 4 * i, [128, 2, 512], F32) for i in range(4)]
            hact = [V(160 + 4 * i, [128, 4, 512], BF16) for i in range(2)]
            xg = [V(168 + 2 * i, [128, 512], F32) for i in range(2)]
            sg = [V(172 + 2 * i, [128, 512], F32) for i in range(2)]
            tl = [V(176 + 2 * i, [128, 512], F32) for i in range(2)]
            xl = [V(180 + 2 * i, [128, 512], F32) for i in range(2)]
            gsx = [V(184 + 2 * i, [128, 512], F32) for i in range(2)]
            bug = sb("bug", [128, NE, 16], F32, e7)
            s_Wu = [[Slot() for _ in range(16)] for _ in range(2)]
            s_Wd = [slots(4) for _ in range(2)]
            s_wst = slots(4)
            s_hact = [slots(4) for _ in range(2)]
            s_xg = slots(2); s_sg = slots(2); s_tl = slots(2); s_xl = slots(2); s_gsx = slots(2)
            pg = [ps("pg%d" % i, [128, 512], F32, e7) for i in range(2)]
            pl = [ps("pl%d" % i, [128, 512], F32, e7) for i in range(2)]
            py = [ps("py%d" % i, [128, 512], F32, e7) for i in range(3)]
            s_pg = slots(2); s_pl = slots(2); s_py = slots(3)
            s_xr = [slots(2) for _ in range(NT)]
            dbug = DMA(P, bug, din("b_ug_col"))
            wug = din("w_up_gate")
            wdn = din("w_down")
            cnt = [0]
            castn = [0]

            def cast_eng():
                castn[0] += 1
                return "act" if castn[0] % 2 == 0 else "pool"

            def load_unit(e, fh):
                ui = (e * 2 + fh) % 2
                for part in range(2):
                    c0 = part * 1024 + fh * 512
                    src = wug[e].rearrange("(kc p) n -> p kc n", p=128)[:, :, c0:c0 + 512]
                    for k0 in range(0, KC, 2):
                        b = cnt[0] % 4
                        cnt[0] += 1
                        DMA(P, wst[b], src[:, k0:k0 + 2, :], w=[s_wst[b]])
                        for j in range(2):
                            kc = k0 + j
                            eng = cast_eng()
                            dst = Wu[ui][:, kc, part * 512:(part + 1) * 512]
                            if eng == "act":
                                ACT(P, dst, wst[b][:, j, :], AF.Copy, r=[s_wst[b]], w=[s_Wu[ui][kc * 2 + part]])
                            else:
                                CP(P, eng, dst, wst[b][:, j, :], r=[s_wst[b]], w=[s_Wu[ui][kc * 2 + part]])
                srcd = wdn[e].rearrange("(fc p) n -> p fc n", p=128)[:, fh * 4:fh * 4 + 4, :]
                for fc in range(4):
                    b = cnt[0] % 4
                    cnt[0] += 1
                    DMA(P, wst[b].rearrange("p a b -> p (a b)"), srcd[:, fc, :], w=[s_wst[b]])
                    eng = cast_eng()
                    if eng == "act":
                        ACT(P, Wd[ui][:, fc, :], wst[b].rearrange("p a b -> p (a b)"), AF.Copy, r=[s_wst[b]],
                            w=[s_Wd[ui][fc]])
                    else:
                        CP(P, eng, Wd[ui][:, fc, :], wst[b].rearrange("p a b -> p (a b)"), r=[s_wst[b]],
                           w=[s_Wd[ui][fc]])

            ngl = [0]
            nhb = [0]
            npy = [0]

            def up(e, fh, tg):
                ui = (e * 2 + fh) % 2
                hb = nhb[0] % 2
                nhb[0] += 1
                for fc in range(4):
                    gi = ngl[0] % 2
                    ngl[0] += 1
                    for part, pp, s_pp in [(0, pg[gi], s_pg[gi]), (1, pl[gi], s_pl[gi])]:
                        for kc in range(KC):
                            o = MM(P, pp, Wu[ui][:, kc, part * 512 + fc * 128: part * 512 + (fc + 1) * 128],
                                   hnT[:, kc, tg * 512:(tg + 1) * 512], kc == 0, kc == KC - 1,
                                   r=[s_Wu[ui][kc * 2 + part]], w=([s_pp] if kc == 0 else []))
                        s_pp.w = o
                    bg = bug[:, e, fh * 4 + fc: fh * 4 + fc + 1]
                    bl = bug[:, e, 8 + fh * 4 + fc: 8 + fh * 4 + fc + 1]
                    TS(P, "dve", xg[gi], pg[gi], bg, LIMIT, ALU.add, ALU.min, r=[s_pg[gi]], w=[s_xg[gi]], x=[dbug])
                    ACT(P, sg[gi], xg[gi], AF.Sigmoid, r=[s_xg[gi]], w=[s_sg[gi]], scale=ALPHA)
                    TS(P, "dve", tl[gi], pl[gi], bl, LIMIT, ALU.add, ALU.min, r=[s_pl[gi]], w=[s_tl[gi]], x=[dbug])
                    TS(P, "dve", xl[gi], tl[gi], -LIMIT, 1.0, ALU.max, ALU.add, r=[s_tl[gi]], w=[s_xl[gi]])
                    TT(P, POOL, gsx[gi], xg[gi], sg[gi], ALU.mult, r=[s_xg[gi], s_sg[gi]], w=[s_gsx[gi]])
                    TT(P, POOL, hact[hb][:, fc, :], gsx[gi], xl[gi], ALU.mult, r=[s_gsx[gi], s_xl[gi]],
                       w=[s_hact[hb][fc]])
                return hb

            def down(e, fh, tg, hb):
                ui = (e * 2 + fh) % 2
                for tt_ in range(4):
                    t = tg * 4 + tt_
                    for half in range(2):
                        yi = npy[0] % 3
                        npy[0] += 1
                        for fc in range(4):
                            o = MM(P, py[yi], hact[hb][:, fc, tt_ * 128:(tt_ + 1) * 128],
                                   Wd[ui][:, fc, half * 512:(half + 1) * 512], fc == 0, fc == 3,
                                   r=[s_hact[hb][fc], s_Wd[ui][fc]], w=([s_py[yi]] if fc == 0 else []))
                        s_py[yi].w = o
                        xr = xres[:, t, half * 512:(half + 1) * 512]
                        STT(P, xr, py[yi], G[:, t, e:e + 1], xr, ALU.mult, ALU.add, r=[s_py[yi]], w=[s_xr[t][half]])

            units = [(e, fh) for e in range(n_exp) for fh in range(2)]
            load_unit(*units[0])
            pend = None
            for ui_, (e, fh) in enumerate(units):
                for tg in range(4):
                    hb = up(e, fh, tg)
                    if tg == 0 and ui_ + 1 < len(units):
                        pass
                    if pend is not None:
                        down(*pend)
                    pend = (e, fh, tg, hb)
                    if tg == 1 and ui_ + 1 < len(units):
                        load_unit(*units[ui_ + 1])
            down(*pend)
            P.emit()

        if debug in ("M", "M2"):
            P = Phase(K, "dbgM")
            fin = [DMA(P, dout("d_x2", [128, NT, D]), xres)]
            finish(P, fin)
            P.emit()
            return nc, dbg

        P = Phase(K, "L")
        with ExitStack() as e8:
            gple = sb("gple", [128, KC], F32, e8)
            Wpg = V(0, [128, KC, D], BF16)
            Wpp = V(16, [128, 2, D], BF16)
            s_Wpg = slots(8); s_Wpp = slots(2)
            wst = [V(96 + 8 * i, [128, 2, D], F32) for i in range(2)]
            s_wst = slots(2)
            xs = [V(112 + 4 * i, [128, D], F32) for i in range(2)]
            junk = V(120, [128, D], F32)
            x3T = [V(124 + 2 * i, [128, KC, 128], BF16) for i in range(2)]
            pt = [V(128 + i, [128, PLE], F32) for i in range(2)]
            pT = [V(130 + 0.5 * i, [128, 2, 128], BF16) for i in range(2)]
            sgm = [V(132 + 2 * i, [128, 512], F32) for i in range(2)]
            tmp = [V(136 + 2 * i, [128, 512], F32) for i in range(2)]
            ot = [V(140 + 4 * i, [128, D], F32) for i in range(2)]
            s_xs = slots(2); s_junk = Slot(); s_x3T = slots(2); s_pt = slots(2); s_pT = slots(2)
            s_sgm = slots(2); s_tmp = slots(2); s_ot = slots(2)
            ssqA = sb("ssqL", [128, NT], F32, e8)
            srtA = sb("srtL", [128, NT], F32, e8)
            rstA = sb("rstL", [128, NT], F32, e8)
            tp = [ps("tpL%d" % i, [128, 4, 128], F32, e8) for i in range(3)]
            s_tp = slots(3)
            junkL = ps("junkL", [128, 512], F32, e8)

            def fillL(k):
                for _ in range(k):
                    MM(P, junkL, identb, Wpg[:, 0, 0:512], True, True, r=[s_Wpg[0]])

            gps = [ps("gps%d" % i, [128, 512], F32, e8) for i in range(2)]
            pps = [ps("pps%d" % i, [128, 512], F32, e8) for i in range(2)]
            s_gps = slots(2); s_pps = slots(2)
            dg = DMA(P, gple, din("gple_col"))
            cnt = [0]
            load_w_bf16(P, Wpg, din("w_ple_gate").rearrange("(kc p) n -> p kc n", p=128), D, 8, wst, s_wst, s_Wpg, cnt,
                        scale_col=None)
            load_w_bf16(P, Wpp, din("w_ple_proj").rearrange("(kc p) n -> p kc n", p=128), D, 2, wst, s_wst, s_Wpp, cnt)
            p_d = din("p")
            tcount = [0]
            fin = []
            nqc = [0]

            def frontL(t):
                i = t % 2

                def evac(kc0, tpa, s_tpa, i=i):
                    o_ = TT(P, "dve", x3T[i][:, kc0:kc0 + 4, :], tpa,
                            gple[:, kc0:kc0 + 4, None].broadcast_to([128, 4, 128]), ALU.mult,
                            r=[s_tpa], w=([s_x3T[i]] if kc0 == 0 else []), x=[dg])
                    if kc0 == 4:
                        s_x3T[i].w = o_

                fillL(4)
                norm_transpose(P, t, xres[:, t, :], None, xs[i], s_xs[i], junk, s_junk, ssqA[:, t:t + 1],
                               srtA[:, t:t + 1], rstA[:, t:t + 1], tp, s_tp, tcount, evac)
                DMA(P, pt[i], p_d[t * 128:(t + 1) * 128, :], w=[s_pt[i]])
                fillL(2)
                pi = tcount[0] % 3
                tcount[0] += 1
                for j in range(2):
                    o = TR(P, tp[pi][:, j, :], pt[i][:, j * 128:(j + 1) * 128], ident, r=[s_pt[i]],
                           w=([s_tp[pi]] if j == 0 else []))
                s_tp[pi].w = o
                CP(P, "dve", pT[i], tp[pi][:, 0:2, :], r=[s_tp[pi]], w=[s_pT[i]])

            def backL(t):
                i = t % 2
                for half in range(2):
                    qi = nqc[0] % 2
                    nqc[0] += 1
                    fillL(3)
                    for kc in range(KC):
                        o = MM(P, gps[qi], x3T[i][:, kc, :], Wpg[:, kc, half * 512:(half + 1) * 512], kc == 0,
                               kc == KC - 1, r=[s_x3T[i], s_Wpg[kc]], w=([s_gps[qi]] if kc == 0 else []))
                    s_gps[qi].w = o
                    for c in range(2):
                        o = MM(P, pps[qi], pT[i][:, c, :], Wpp[:, c, half * 512:(half + 1) * 512], c == 0, c == 1,
                               r=[s_pT[i], s_Wpp[c]], w=([s_pps[qi]] if c == 0 else []))
                    s_pps[qi].w = o
                    ACT(P, sgm[qi], gps[qi], AF.Sigmoid, r=[s_gps[qi]], w=[s_sgm[qi]])
                    TT(P, "dve", tmp[qi], pps[qi], sgm[qi], ALU.mult, r=[s_pps[qi], s_sgm[qi]], w=[s_tmp[qi]])
                    TT(P, POOL, ot[i][:, half * 512:(half + 1) * 512], tmp[qi],
                       xres[:, t, half * 512:(half + 1) * 512], ALU.add, r=[s_tmp[qi]],
                       w=([s_ot[i]] if half == 0 else []))
                s_ot[i].w = P.ops["pool"][-1]
                fin.append(DMA(P, out_d[t * 128:(t + 1) * 128, :], ot[i], r=[s_ot[i]]))

            frontL(0)
            for t in range(NT):
                if t + 1 < NT:
                    frontL(t + 1)
                backL(t)
            finish(P, fin)
            P.emit()

    return nc, dbg


def _consts():
    c = {}
    c["c_ident"] = np.eye(128, dtype=np.float32)
    half = HD // 2
    invf = (10000.0 ** (-np.arange(half, dtype=np.float32) / half)).astype(np.float32)
    c["c_invf"] = np.ascontiguousarray(np.broadcast_to(invf[None, :], (128, 32))).astype(np.float32)
    log_g = np.log(1.0 - 2.0 ** (-5.0 - np.arange(NH, dtype=np.float64)))
    idx = np.arange(128, dtype=np.float64)
    c["c_qdec"] = np.exp(log_g[None, :] * (idx[:, None] + 1.0)).astype(np.float32)
    c["c_kinv"] = np.exp(-log_g[None, :] * (idx[:, None] + 1.0)).astype(np.float32)
    gc = np.exp(log_g * 128.0)
    gcb = np.zeros((128, 4, 64), np.float32)
    for h in range(NH):
        gcb[(h % 2) * 64:(h % 2) * 64 + 64, h // 2, :] = gc[h]
    c["c_gcb"] = gcb
    jj, ii = np.meshgrid(np.arange(128), np.arange(128), indexing="ij")
    c["c_retmask"] = (ii >= jj).astype(np.float32)
    c["c_negtri"] = -(jj >= ii).astype(np.float32)
    c["c_negones"] = -np.ones((128, 128), np.float32)
    m = np.zeros((128, 4, 512), np.float32)
    kk = np.arange(128)[:, None]
    qq = np.arange(512)[None, :]
    for i in range(4):
        m[:, i, :] = np.where(kk + 128 * i < qq, 0.0, -30000.0)
    c["c_sbmask"] = m
    return c


def _col(v):
    return np.ascontiguousarray(np.asarray(v, np.float32).reshape(-1, 128).T)


def _bc(v, reps=1):
    v = np.tile(np.asarray(v, np.float32), reps)
    return np.ascontiguousarray(np.broadcast_to(v[None, :], (128, v.shape[0])))


def make_in_maps(x, p, positions, g_mix_norm, w_in, sb_q_gain, sb_k_gain, ret_q_gain, ret_k_gain,
                 ret_gn_gain, ret_gn_bias, w_out, g_ffn_norm, w_router, b_router, w_up_gate, b_up_gate,
                 w_down, b_down, g_ple_norm, w_ple_gate, w_ple_proj):
    f = lambda a: np.ascontiguousarray(np.asarray(a, np.float32))
    shared = dict(_consts())
    shared["gmix_col"] = _col(g_mix_norm[0])
    shared["w_in"] = f(w_in[0])
    shared["gains_bc"] = np.ascontiguousarray(np.stack(
        [_bc(sb_q_gain[0], 8), _bc(sb_k_gain[0], 8), _bc(ret_q_gain[0], 8), _bc(ret_k_gain[0], 8)], axis=1))
    shared["gn_bc"] = np.ascontiguousarray(np.stack([_bc(ret_gn_gain[0]), _bc(ret_gn_bias[0])], axis=1))
    shared["w_out"] = f(w_out[0])
    shared["gffn_col"] = _col(g_ffn_norm[0])
    shared["w_router"] = f(w_router[0])
    shared["b_router_bc"] = _bc(b_router[0])
    shared["w_up_gate"] = f(w_up_gate[0])
    shared["b_ug_col"] = np.ascontiguousarray(
        np.asarray(b_up_gate[0], np.float32).reshape(NE, 16, 128).transpose(2, 0, 1))
    shared["w_down"] = f(w_down[0])
    shared["b_down"] = f(b_down[0])
    shared["gple_col"] = _col(g_ple_norm[0])
    shared["w_ple_gate"] = f(w_ple_gate[0])
    shared["w_ple_proj"] = f(w_ple_proj[0])
    maps = []
    xs = np.asarray(x, np.float32)
    ps_ = np.asarray(p, np.float32)
    pos = np.asarray(positions).astype(np.int32)
    for b in range(8):
        m = dict(shared)
        m["x"] = np.ascontiguousarray(xs[b])
        m["p"] = np.ascontiguousarray(ps_[0, b])
        m["pos_col"] = np.ascontiguousarray(pos[b].reshape(NT, 128).T)
        maps.append(m)
    return maps


def kernel(**inputs):
    nc, info = build_nc()
    maps = make_in_maps(**inputs)
    maps = [{k: v for k, v in m.items() if k in info["_declared"]} for m in maps]
    res = run_bass_kernel_spmd(nc, maps, core_ids=list(range(8)))
    out = np.stack([np.asarray(r["out"], np.float32) for r in res.results], axis=0)
    return out
```
